# Optimizing a Trainium2 kernel written in Bass

```python
import math
import jax, jax.numpy as jnp
from jax import lax
import numpy as np

D_MODEL = 2048
BATCH = 16
SEQ = 256
DEPTH = 2
DEC_BATCH = 4
DEC_SEQ = 4096
PAST_LEN = 512

GRID_W = 64
N_HEADS = 8
HEAD_DIM = D_MODEL // (2 * N_HEADS)
V_DIM = 2 * HEAD_DIM
ROPE_BASE = 10000.0
Q_BLOCK = 128
N_POOL_GROUPS = 4
POOL_WINDOWS = (2, 4, 8, 16)
POOL_GROUP_DIM = D_MODEL // N_POOL_GROUPS
D_FF = 11 * D_MODEL // 4
N_EXPERTS = 8
TOP_K = 2
D_FF_EXPERT = D_FF // 2
N_EVEN = (DEPTH + 1) // 2
N_ODD = DEPTH // 2
EPS = 1e-6

kernel_name = "diffattn_pool_moe_context_prefix_trunk"

F32 = jnp.float32


def rms_norm(x, g):
    xf = x.astype(F32)
    y = xf * lax.rsqrt(jnp.mean(xf * xf, axis=-1, keepdims=True) + EPS)
    return (y * g.astype(F32)).astype(x.dtype)


def adaln_mod(cond, w, b):
    m = (jax.nn.silu(cond) @ w + b)[:, None, :]
    return jnp.split(m, 6, axis=-1)


def modulate(h, shift, scale):
    return h * (1 + scale) + shift


def axial_rope_tables(rows):
    r = jnp.repeat(jnp.arange(rows, dtype=F32), GRID_W)
    col = jnp.tile(jnp.arange(GRID_W, dtype=F32), rows)
    n_freq = HEAD_DIM // 4
    freqs = ROPE_BASE ** (-jnp.arange(n_freq, dtype=F32) / n_freq)
    ang = jnp.stack([r[:, None] * freqs, col[:, None] * freqs], axis=1)
    return jnp.cos(ang), jnp.sin(ang)


def apply_axial_rope(x, cos, sin):
    xr = x.astype(F32).reshape(*x.shape[:-1], 2, 2, HEAD_DIM // 4)
    x1, x2 = xr[..., 0, :], xr[..., 1, :]
    cs, sn = cos[:, None, None], sin[:, None, None]
    out = jnp.stack([x1 * cs - x2 * sn, x2 * cs + x1 * sn], axis=-2)
    return out.reshape(x.shape).astype(x.dtype)


def diff_qkv(h, w_qkv, q_g, k_g):
    b, l, _ = h.shape
    q, k, v = jnp.split(h @ w_qkv, 3, axis=-1)
    q = rms_norm(q.reshape(b, l, N_HEADS, 2, HEAD_DIM), q_g)
    k = rms_norm(k.reshape(b, l, N_HEADS, 2, HEAD_DIM), k_g)
    return q, k, v.reshape(b, l, N_HEADS, V_DIM)


def diff_lambda(lp, lambda_init):
    lp = lp.astype(F32)
    return jnp.exp(jnp.sum(lp[0] * lp[1])) - jnp.exp(jnp.sum(lp[2] * lp[3])) + lambda_init


def diff_attend(q, k, v, lam):
    s = jnp.einsum('bqhcd,bkhcd->bhcqk', q, k).astype(F32) * (HEAD_DIM ** -0.5)
    p = jax.nn.softmax(s, axis=-1)
    a = p[:, :, 0] - lam * p[:, :, 1]
    return jnp.einsum('bhqk,bkhe->bqhe', a.astype(v.dtype), v)


def diff_attend_blocked(q, k, v, lam):
    b, l = q.shape[:2]
    nb = l // Q_BLOCK
    qb = jnp.moveaxis(q.reshape(b, nb, Q_BLOCK, N_HEADS, 2, HEAD_DIM), 1, 0)
    ob = lax.map(lambda qq: diff_attend(qq, k, v, lam), qb)
    return jnp.moveaxis(ob, 0, 1).reshape(b, l, N_HEADS, V_DIM)


def diff_out(o, subln_g, lambda_init, w_o):
    b, l = o.shape[:2]
    o = rms_norm(o, subln_g) * (1.0 - lambda_init)
    return o.reshape(b, l, D_MODEL) @ w_o


def pool_mixer(h, w_groups, scale):
    b, l, _ = h.shape
    hf = h.astype(F32).reshape(b, l, N_POOL_GROUPS, POOL_GROUP_DIM)
    csum = jnp.concatenate([jnp.zeros((b, 1, N_POOL_GROUPS, POOL_GROUP_DIM), F32),
                            jnp.cumsum(hf, axis=1)], axis=1)
    t = jnp.arange(l)
    means = []
    for g, w in enumerate(POOL_WINDOWS):
        lo = jnp.clip(t - w // 2, 0, l - 1)
        hi = jnp.clip(t + w // 2 - 1, 0, l - 1)
        cnt = (hi - lo + 1).astype(F32)
        s = csum[:, hi + 1, g] - csum[:, lo, g]
        means.append(s / cnt[None, :, None])
    pooled = (jnp.stack(means, axis=2) - hf).astype(h.dtype)
    y = jnp.einsum('blgc,gce->blge', pooled, w_groups).reshape(b, l, D_MODEL)
    return y * scale


def swiglu(h, w_gu, w_down):
    g, u = jnp.split(h @ w_gu, 2, axis=-1)
    return (jax.nn.silu(g) * u) @ w_down


def moe_swiglu(h, w_router, b_router, w_gu, w_down):
    shp = h.shape
    t = h.reshape(-1, D_MODEL)
    logits = (t @ w_router).astype(F32) + b_router.astype(F32)
    top_v, top_i = lax.top_k(logits, TOP_K)
    gates = jax.nn.softmax(top_v, axis=-1)
    combine = jnp.einsum('tk,tke->te', gates, jax.nn.one_hot(top_i, N_EXPERTS, dtype=F32))
    out = jnp.zeros(t.shape, F32)
    for e in range(N_EXPERTS):
        out = out + combine[:, e:e + 1] * swiglu(t, w_gu[e], w_down[e]).astype(F32)
    return out.astype(h.dtype).reshape(shp)


def setup_inputs(seed: int = 0) -> dict:
    key = jax.random.key(seed)
    ks = jax.random.split(key, 24)

    def nrm(k, shape, s):
        return jax.random.normal(k, shape, F32) * s

    return {
        "x_prompt": nrm(ks[0], (BATCH, SEQ, D_MODEL), 1.0),
        "x_sample": nrm(ks[1], (DEC_BATCH, DEC_SEQ, D_MODEL), 1.0),
        "c": nrm(ks[2], (DEC_BATCH, D_MODEL), 1.0),
        "cache_k": nrm(ks[3], (DEC_BATCH, N_EVEN, PAST_LEN, N_HEADS, V_DIM), 1.0),
        "cache_v": nrm(ks[4], (DEC_BATCH, N_EVEN, PAST_LEN, N_HEADS, V_DIM), 1.0),
        "c_ctx": nrm(ks[5], (D_MODEL,), 1.0),
        "ada_w": nrm(ks[6], (DEPTH, D_MODEL, 6 * D_MODEL), 0.5 * D_MODEL ** -0.5),
        "ada_b": nrm(ks[7], (DEPTH, 6 * D_MODEL), 0.02),
        "norm1_g": 1.0 + nrm(ks[8], (DEPTH, D_MODEL), 0.02),
        "norm2_g": 1.0 + nrm(ks[9], (DEPTH, D_MODEL), 0.02),
        "attn_w_qkv": nrm(ks[10], (N_EVEN, D_MODEL, 3 * D_MODEL), D_MODEL ** -0.5),
        "attn_w_o": nrm(ks[11], (N_EVEN, D_MODEL, D_MODEL), D_MODEL ** -0.5),
        "attn_q_norm": 1.0 + nrm(ks[12], (N_EVEN, HEAD_DIM), 0.02),
        "attn_k_norm": 1.0 + nrm(ks[13], (N_EVEN, HEAD_DIM), 0.02),
        "attn_lambda": nrm(ks[14], (N_EVEN, 4, HEAD_DIM), 0.1),
        "attn_subln_g": 1.0 + nrm(ks[15], (N_EVEN, V_DIM), 0.02),
        "pool_w": nrm(ks[16], (N_ODD, N_POOL_GROUPS, POOL_GROUP_DIM, POOL_GROUP_DIM), POOL_GROUP_DIM ** -0.5),
        "pool_scale": 1.0 + nrm(ks[17], (N_ODD, D_MODEL), 0.1),
        "ffn_w_gu": nrm(ks[18], (N_EVEN, D_MODEL, 2 * D_FF), D_MODEL ** -0.5),
        "ffn_w_down": nrm(ks[19], (N_EVEN, D_FF, D_MODEL), D_FF ** -0.5),
        "moe_w_router": nrm(ks[20], (N_ODD, D_MODEL, N_EXPERTS), D_MODEL ** -0.5),
        "moe_b_router": nrm(ks[21], (N_ODD, N_EXPERTS), 0.01),
        "moe_w_gu": nrm(ks[22], (N_ODD, N_EXPERTS, D_MODEL, 2 * D_FF_EXPERT), D_MODEL ** -0.5),
        "moe_w_down": nrm(ks[23], (N_ODD, N_EXPERTS, D_FF_EXPERT, D_MODEL), D_FF_EXPERT ** -0.5),
    }


def reference(x_prompt, x_sample, c, cache_k, cache_v, c_ctx, ada_w, ada_b, norm1_g, norm2_g,
              attn_w_qkv, attn_w_o, attn_q_norm, attn_k_norm, attn_lambda, attn_subln_g,
              pool_w, pool_scale, ffn_w_gu, ffn_w_down,
              moe_w_router, moe_b_router, moe_w_gu, moe_w_down):
    rows = x_sample.shape[1] // GRID_W
    cos, sin = axial_rope_tables(rows)
    b_ctx, l_ctx = x_prompt.shape[:2]
    b_dec, l_past = cache_k.shape[0], cache_k.shape[2]
    xp, xs = x_prompt, x_sample
    new_k, new_v = [], []
    for i in range(DEPTH):
        j = i // 2
        p_sh1, p_sc1, p_g1, p_sh2, p_sc2, p_g2 = adaln_mod(c_ctx[None, :], ada_w[i], ada_b[i])
        s_sh1, s_sc1, s_g1, s_sh2, s_sc2, s_g2 = adaln_mod(c, ada_w[i], ada_b[i])
        hp = modulate(rms_norm(xp, norm1_g[i]), p_sh1, p_sc1)
        hs = modulate(rms_norm(xs, norm1_g[i]), s_sh1, s_sc1)
        if i % 2 == 0:
            lambda_init = 0.8 - 0.6 * math.exp(-0.3 * i)
            lam = diff_lambda(attn_lambda[j], lambda_init)
            qp, kp, vp = diff_qkv(hp, attn_w_qkv[j], attn_q_norm[j], attn_k_norm[j])
            mp = diff_out(diff_attend(qp, kp, vp, lam), attn_subln_g[j], lambda_init, attn_w_o[j])
            new_k.append(kp.reshape(b_ctx, l_ctx, N_HEADS, V_DIM))
            new_v.append(vp)
            qs, ks_, vs = diff_qkv(hs, attn_w_qkv[j], attn_q_norm[j], attn_k_norm[j])
            qs = apply_axial_rope(qs, cos, sin)
            ks_ = apply_axial_rope(ks_, cos, sin)
            k_all = jnp.concatenate(
                [cache_k[:, j].reshape(b_dec, l_past, N_HEADS, 2, HEAD_DIM).astype(ks_.dtype), ks_], axis=1)
            v_all = jnp.concatenate([cache_v[:, j].astype(vs.dtype), vs], axis=1)
            ms = diff_out(diff_attend_blocked(qs, k_all, v_all, lam), attn_subln_g[j], lambda_init, attn_w_o[j])
        else:
            mp = pool_mixer(hp, pool_w[j], pool_scale[j])
            ms = pool_mixer(hs, pool_w[j], pool_scale[j])
        xp = xp + p_g1 * mp
        xs = xs + s_g1 * ms
        hp = modulate(rms_norm(xp, norm2_g[i]), p_sh2, p_sc2)
        hs = modulate(rms_norm(xs, norm2_g[i]), s_sh2, s_sc2)
        if i % 2 == 0:
            fp = swiglu(hp, ffn_w_gu[j], ffn_w_down[j])
            fs = swiglu(hs, ffn_w_gu[j], ffn_w_down[j])
        else:
            fp = moe_swiglu(hp, moe_w_router[j], moe_b_router[j], moe_w_gu[j], moe_w_down[j])
            fs = moe_swiglu(hs, moe_w_router[j], moe_b_router[j], moe_w_gu[j], moe_w_down[j])
        xp = xp + p_g2 * fp
        xs = xs + s_g2 * fs
    state_k = jnp.stack(new_k, axis=1)
    state_v = jnp.stack(new_v, axis=1)
    return (xp, xs, state_k, state_v)
```

```python
import math
import os
import numpy as np
from contextlib import ExitStack
import concourse.bass as bass
import concourse.mybir as mybir
from concourse.bass_utils import run_bass_kernel_spmd

F32 = mybir.dt.float32
BF16 = mybir.dt.bfloat16
U8 = mybir.dt.uint8
AF = mybir.ActivationFunctionType
ALU = mybir.AluOpType
AX = mybir.AxisListType

D = 2048
NH = 8
HD = 128
DFF = 5632
DFE = 2816
NE = 8
EPS = 1e-6
NPT = 4
NSO = 16
NSQ = 17
NSA = 32
PAST = 512
NKEY = PAST + NSA * 128
LAMBDA_INIT = 0.8 - 0.6 * math.exp(-0.3 * 0)
ARENA_BYTES = 206 * 1024
MAXPH = int(os.environ.get('KMAXPH', '7'))
_DECL = []
KDEBUG = [x for x in os.environ.get('KDEBUG', '').split(',') if x]
_DBG = []
KSKIP0 = bool(int(os.environ.get('KSKIP0', '0')))
KABLK = int(os.environ.get('KABLK', '99'))
KASKIP = [x for x in os.environ.get('KASKIP', '').split(',') if x]


class Buf:
    __slots__ = ("name", "w", "r")

    def __init__(self, name):
        self.name = name
        self.w = {}
        self.r = {}


class T:
    __slots__ = ("ap", "b", "name", "excl")

    def __init__(self, ap, name, excl=False):
        self.ap = ap
        self.b = Buf(name)
        self.name = name
        self.excl = excl


class Sched:
    ENGS = ("pe", "act", "dve", "pool", "sp")

    def __init__(self, nc, stack):
        self.nc = nc
        self.stack = stack
        self.ops = {e: [] for e in self.ENGS}
        self.sems = {}
        self.waited = {e: {} for e in self.ENGS}
        self.dma_map = {}
        self.nsem = 0
        for e in self.ENGS:
            self._sem("prog_" + e)

    def _sem(self, key):
        if key not in self.sems:
            h = self.stack.enter_context(self.nc.semaphore(key))
            self.sems[key] = [h, 0]
            self.nsem += 1
        return self.sems[key]

    def op(self, eng, body, reads=(), writes=(), dma=None):
        ex = [t for t in reads if t.excl]
        if ex:
            reads = [t for t in reads if not t.excl]
            writes = list(writes) + [t for t in ex if t not in writes]
        waits = {}
        for t in reads:
            for k, v in t.b.w.items():
                if waits.get(k, 0) < v:
                    waits[k] = v
        for t in writes:
            for d in (t.b.w, t.b.r):
                for k, v in d.items():
                    if waits.get(k, 0) < v:
                        waits[k] = v
        wl = []
        wd = self.waited[eng]
        for k, v in waits.items():
            if wd.get(k, 0) < v:
                wd[k] = v
                wl.append((self.sems[k][0], v))
        if dma is None:
            key = "prog_" + eng
            amt = 1
        else:
            dk = (eng, dma)
            if dk not in self.dma_map:
                n = sum(1 for k in self.dma_map if k[0] == eng)
                self.dma_map[dk] = "dq%s%d" % (eng, n)
            key = self.dma_map[dk]
            amt = 16
        s = self._sem(key)
        s[1] += amt
        ev = s[1]
        self.ops[eng].append((wl, body, s[0], amt))
        for t in reads:
            if t.b.r.get(key, 0) < ev:
                t.b.r[key] = ev
        for t in writes:
            t.b.w = {key: ev}
            t.b.r = {}

    def barrier(self):
        snap = {k: v[1] for k, v in self.sems.items() if v[1] > 0}
        for e in self.ENGS:
            wd = self.waited[e]
            wl = []
            for k, v in snap.items():
                if wd.get(k, 0) < v:
                    wd[k] = v
                    wl.append((self.sems[k][0], v))
            s = self.sems["prog_" + e]
            s[1] += 1
            self.ops[e].append((wl, (lambda eng: eng.nop()), s[0], 1))
        self.dma_map = {}

    def emit(self):
        ops = self.ops

        def replay(name):
            def f(eng):
                for wl, body, sem, amt in ops[name]:
                    for (h, v) in wl:
                        eng.wait_ge(h, v)
                    ins = body(eng)
                    ins.then_inc(sem, amt)
            return f

        with self.nc.Block() as block:
            block.tensor(replay("pe"))
            block.scalar(replay("act"))
            block.vector(replay("dve"))
            block.gpsimd(replay("pool"))
            block.sync(replay("sp"))


class Arena:
    def __init__(self, nc):
        self.t = nc.alloc_sbuf_tensor("arena", [128, ARENA_BYTES], U8)
        self.off = 0
        self.n = 0

    def reset(self, to=0):
        self.off = to

    def alloc(self, name, shape, dtype):
        esz = 4 if dtype == F32 else 2
        nbytes = int(np.prod(shape[1:])) * esz
        assert self.off + nbytes <= ARENA_BYTES, (name, self.off, nbytes)
        a = self.t[:, self.off:self.off + nbytes].bitcast(dtype)
        self.off += (nbytes + 63) // 64 * 64
        if len(shape) == 3:
            a = a.rearrange("p (a b) -> p a b", a=shape[1])
        elif len(shape) == 4:
            a = a.rearrange("p (a b c) -> p a b c", a=shape[1], b=shape[2])
        self.n += 1
        return T(a, "%s_%d" % (name, self.n))


def build_program():
    nc = bass.Bass("TRN2", target_bir_lowering=False)

    def din(name, shape, dt=F32, minph=0):
        if MAXPH < minph:
            return None
        _DECL.append(name)
        return nc.dram_tensor(name, shape, dt, kind="ExternalInput").ap()

    def dout(name, shape):
        return nc.dram_tensor(name, shape, F32, kind="ExternalOutput").ap()

    def dscr(name, shape, dt):
        if name in KDEBUG:
            _DBG.append(name)
            return nc.dram_tensor(name, shape, dt, kind="ExternalOutput").ap()
        return nc.dram_tensor(name, shape, dt, kind="Internal").ap()

    xp = din("xp", [512, D])
    xs = din("xs", [4096, D])
    condT = din("condT", [128, 32])
    ck = din("ck", [PAST, D])
    cv = din("cv", [PAST, D])
    ropeC = din("ropeC", [4096, 128])
    ropeS = din("ropeS", [4096, 128])
    ada_w = din("ada_w", [2, D, 6 * D], minph=(99 if KSKIP0 else 0))
    ada_b = din("ada_b", [2, 6 * D])
    n1g = din("n1g", [2, D])
    n2g = din("n2g", [2, D])
    wqkv = din("wqkv", [D, 3 * D], minph=1)
    wo = din("wo", [D, D], minph=3)
    qng = din("qng", [1, HD])
    kng = din("kng", [1, HD])
    lamp = din("lamp", [1, 4 * HD])
    subg = din("subg", [1, 256])
    poolw = din("poolw", [4, 512, 512])
    pools = din("pools", [1, D])
    wgu = din("wgu", [D, 2 * DFF], minph=4)
    wdn = din("wdn", [DFF, D], minph=4)
    wrT = din("wrT", [NE, D])
    br = din("br", [1, NE])
    mgu = din("mgu", [NE, D, 2 * DFE], minph=7)
    mdn = din("mdn", [NE, DFE, D], minph=7)
    ident = din("ident", [128, 128])
    apool = din("apool", [20, 128, 3 * 4 * 128])

    yp = dout("yp", [512, D])
    ys = dout("ys", [2048, D])
    sk = dout("sk", [512, D])
    sv = dout("sv", [512, D])

    mods = din("mods_in", [2, 2, 6 * D]) if KSKIP0 else dscr("mods", [2, 2, 6 * D], F32)
    kT = dscr("kT", [16, 128, NKEY], BF16)
    kTp = dscr("kTp", [16, 128, 512], BF16)
    qT = dscr("qT", [16, 128, NSQ * 128], BF16)
    qTp = dscr("qTp", [16, 128, 512], BF16)
    vall = dscr("vall", [NKEY, D], BF16)
    vp = dscr("vp", [512, D], BF16)
    oall = dscr("oall", [21 * 128, D], BF16)
    x1h = dscr("x1h", [128, D], F32)
    h1all = dscr("h1all", [21 * 128, D], BF16)
    combd = dscr("combd", [20 * 128, NE], F32)

    stack = ExitStack()
    with stack:
        S = Sched(nc, stack)
        A = Arena(nc)
        psf = []
        for i in range(8):
            p = nc.alloc_psum_tensor("ps%d" % i, [128, 512], F32)
            psf.append(T(p[:], "ps%d" % i, excl=True))

        def ps16(i):
            return psf[i].ap.bitcast(BF16)

        def V(eng, method, reads, writes, **kw):
            S.op(eng, (lambda e, m=method, kw=kw: getattr(e, m)(**kw)), reads=reads, writes=writes)

        def G(eng, insts, reads, writes):
            def body(e, insts=insts):
                ins = None
                for m, kw in insts:
                    ins = getattr(e, m)(**kw)
                return ins
            S.op(eng, body, reads=reads, writes=writes)

        def dma(eng, out_ap, in_ap, reads, writes, key, **kw):
            S.op(eng, (lambda e, o=out_ap, i=in_ap, kw=kw: e.dma_start(out=o, in_=i, **kw)),
                 reads=reads, writes=writes, dma=key)

        def rstd_from_ss(ss, rs, scale):
            V("dve", "tensor_scalar", [ss], [rs], out=rs.ap, in0=ss.ap, scalar1=scale, scalar2=EPS, op0=ALU.mult, op1=ALU.add)
            V("act", "sqrt", [rs], [rs], out=rs.ap, in_=rs.ap)
            V("dve", "reciprocal", [rs], [rs], out=rs.ap, in_=rs.ap)

        def norm_mod(xt, junk, ss, rs, Ab, shb, out_t):
            V("act", "activation", [xt], [junk, ss], out=junk.ap, in_=xt.ap, func=AF.Square, accum_out=ss.ap)
            rstd_from_ss(ss, rs, 1.0 / D)
            V("dve", "scalar_tensor_tensor", [xt, rs, Ab], [xt], out=xt.ap, in0=xt.ap, scalar=rs.ap, in1=Ab.ap, op0=ALU.mult, op1=ALU.mult)
            V("dve", "tensor_tensor", [xt, shb], [out_t], out=out_t.ap, in0=xt.ap, in1=shb.ap, op=ALU.add)

        def transpose16(src, dstT, col0, pbanks, dst_toks):
            for half in range(2):
                pb = pbanks[half]
                insts = []
                for j in range(8):
                    kc = half * 8 + j
                    insts.append(("transpose", dict(out=ps16(pb)[:, j * 128:(j + 1) * 128], in_=src.ap[:, kc * 128:(kc + 1) * 128], identity=idb.ap)))
                G("pe", insts, [src, idb], [psf[pb]])
                o = dstT.ap[:, half * 8:(half + 1) * 8, col0:col0 + 128]
                i = ps16(pb).rearrange("p (k t) -> p k t", k=8)
                if half == 0:
                    V("dve", "tensor_copy", [psf[pb]], [dst_toks[0]], out=o, in_=i)
                else:
                    V("act", "copy", [psf[pb]], [dst_toks[1]], out=o, in_=i)

        def load_bcast(dst, src_row_ap, key):
            dma("sp", dst.ap, src_row_ap.partition_broadcast(128), [], [dst], key)

        def make_Ab(dst, tmp, sc_row, g_row, key):
            load_bcast(dst, sc_row, key)
            load_bcast(tmp, g_row, key + "g")
            V("dve", "scalar_tensor_tensor", [dst, tmp], [dst], out=dst.ap, in0=dst.ap, scalar=1.0, in1=tmp.ap, op0=ALU.add, op1=ALU.mult)

        def modrow(L, c, idx):
            return mods[L, c:c + 1, idx * D:(idx + 1) * D]

        def resid(i):
            if i < 4:
                return yp[i * 128:(i + 1) * 128, :]
            if i < 20:
                return ys[(i - 4) * 128:(i - 3) * 128, :]
            return x1h

        def xin(i):
            if i < 4:
                return xp[i * 128:(i + 1) * 128, :]
            return xs[(i - 4) * 128:(i - 3) * 128, :]

        def wview(ap2d):
            return ap2d.rearrange("(k p) n -> p k n", p=128)

        idf = A.alloc("idf", [128, 128], F32)
        idb = A.alloc("idb", [128, 128], BF16)
        dma("sp", idf.ap, ident, [], [idf], "idf")
        V("dve", "tensor_copy", [idf], [idb], out=idb.ap, in_=idf.ap)
        base0 = A.off

        def phase0():
            A.reset(base0)
            cT = A.alloc("cT", [128, 32], F32)
            cb = [A.alloc("cb%d" % c, [128, 16, 128], BF16) for c in range(2)]
            dma("sp", cT.ap, condT, [], [cT], "cT")
            V("act", "activation", [cT], [cT], out=cT.ap, in_=cT.ap, func=AF.Silu)
            for c in range(2):
                V("dve", "tensor_copy", [cT], [cb[c]], out=cb[c].ap, in_=cT.ap[:, c * 16:(c + 1) * 16].unsqueeze(2).to_broadcast([128, 16, 128]))
            wst = [A.alloc("adaw%d" % i, [128, 16, 512], BF16) for i in range(2)]
            bst = [A.alloc("adab%d" % i, [128, 512], F32) for i in range(2)]
            mst = [A.alloc("adam%d" % i, [128, 512], F32) for i in range(4)]
            it = 0
            for L in range(2):
                for n in range(24):
                    w_t = wst[it % 2]
                    b_t = bst[it % 2]
                    dma("pool", w_t.ap, wview(ada_w[L, :, n * 512:(n + 1) * 512]), [], [w_t], "adaw%d" % (it % 2))
                    load_bcast(b_t, ada_b[L:L + 1, n * 512:(n + 1) * 512], "adab%d" % (it % 2))
                    for c in range(2):
                        pb = (it * 2 + c) % 4
                        insts = [("matmul", dict(out=psf[pb].ap, lhsT=cb[c].ap[:, kc, :], rhs=w_t.ap[:, kc, :], start=(kc == 0), stop=(kc == 15))) for kc in range(16)]
                        G("pe", insts, [cb[c], w_t], [psf[pb]])
                        m_t = mst[pb]
                        V("dve", "tensor_tensor", [psf[pb], b_t], [m_t], out=m_t.ap, in0=psf[pb].ap, in1=b_t.ap, op=ALU.add)
                        dma("sp", mods[L, c:c + 1, n * 512:(n + 1) * 512], m_t.ap[0:1, :], [m_t], [], "adam%d" % pb)
                    it += 1
            S.barrier()

        def phaseA():
            A.reset(base0)
            Ab = [A.alloc("A1b%d" % c, [128, D], F32) for c in range(2)]
            shb = [A.alloc("sh1b%d" % c, [128, D], F32) for c in range(2)]
            gq = A.alloc("gq", [128, HD], F32)
            gk = A.alloc("gk", [128, HD], F32)
            xt_s = [A.alloc("xt%d" % i, [128, D], F32) for i in range(2)]
            junk = A.alloc("junk", [128, D], BF16)
            hb_s = [A.alloc("hb%d" % i, [128, D], BF16) for i in range(2)]
            ss_s = [A.alloc("ss%d" % i, [128, 1], F32) for i in range(2)]
            rs_s = [A.alloc("rs%d" % i, [128, 1], F32) for i in range(2)]
            TB = 9
            hT = A.alloc("hT", [128, 16, TB * 128], BF16)
            hTtok = [[T(None, "hT_%d_%d" % (j, q)) for q in range(2)] for j in range(TB)]
            wq_s = [A.alloc("wq%d" % i, [128, 16, 512], BF16) for i in range(2)]
            rc_t = A.alloc("rc", [128, TB, 128], F32)
            rsn_t = A.alloc("rsn", [128, TB, 128], F32)
            e1 = [A.alloc("e1_%d" % i, [128, 512], F32) for i in range(2)]
            e2 = [A.alloc("e2_%d" % i, [128, 512], F32) for i in range(2)]
            e3 = [A.alloc("e3_%d" % i, [128, 512], F32) for i in range(2)]
            e4 = [A.alloc("e4_%d" % i, [128, 512], BF16) for i in range(2)]
            ess = [A.alloc("ess%d" % i, [128, 4], F32) for i in range(2)]
            stg = [A.alloc("stg%d" % i, [128, 4, TB * 128], BF16) for i in range(2)]
            vst = [A.alloc("vst%d" % i, [128, 512], BF16) for i in range(2)]
            vsf = [A.alloc("vsf%d" % i, [128, 512], F32) for i in range(2)]
            make_Ab(Ab[0], xt_s[0], modrow(0, 0, 1), n1g[0:1, :], "A1b0")
            make_Ab(Ab[1], xt_s[1], modrow(0, 1, 1), n1g[0:1, :], "A1b1")
            load_bcast(shb[0], modrow(0, 0, 0), "sh1b0")
            load_bcast(shb[1], modrow(0, 1, 0), "sh1b1")
            load_bcast(gq, qng, "gq")
            load_bcast(gk, kng, "gk")

            for t4 in range(0 if "cache" in KASKIP else 4):
                xt = xt_s[t4 % 2]
                hb = hb_s[t4 % 2]
                vb = e4[t4 % 2]
                for q4 in range(4):
                    dma("pool", vb.ap, cv[t4 * 128:(t4 + 1) * 128, q4 * 512:(q4 + 1) * 512], [], [vb], "cvl%d" % (t4 % 2))
                    dma("sp", vall[t4 * 128:(t4 + 1) * 128, q4 * 512:(q4 + 1) * 512], vb.ap, [vb], [], "cvs%d" % (t4 % 2))
                dma("sp", xt.ap, ck[t4 * 128:(t4 + 1) * 128, :], [], [xt], "xt%d" % (t4 % 2))
                V("dve", "tensor_copy", [xt], [hb], out=hb.ap, in_=xt.ap)
                transpose16(hb, hT, 0, (0, 1), hTtok[0])
                dma("sp", kT[:, :, t4 * 128:(t4 + 1) * 128].rearrange("g d t -> d g t"), hT.ap[:, :, 0:128], hTtok[0], [], "ckst")

            tilesA = []
            for i in range(4):
                tilesA.append(dict(kind="p", idx=i, x=xp[i * 128:(i + 1) * 128, :], c=0, q=True))
            for i in range(NSA):
                tilesA.append(dict(kind="s", idx=i, x=xs[i * 128:(i + 1) * 128, :], c=1, q=(i < NSQ)))
            nblk = min(len(tilesA) // TB, KABLK)
            wit = 0
            eit = 0
            tit = 0
            for blk in range(nblk):
                tl = tilesA[blk * TB:(blk + 1) * TB]
                js = [j for j, t in enumerate(tl) if t["kind"] == "s"]
                if js:
                    j0 = js[0]
                    r0 = tl[j0]["idx"] * 128
                    ns = len(js)
                    dma("sp", rc_t.ap[:, j0:j0 + ns, :], ropeC[r0:r0 + ns * 128, :].rearrange("(j p) f -> p j f", p=128), [], [rc_t], "rc")
                    dma("sp", rsn_t.ap[:, j0:j0 + ns, :], ropeS[r0:r0 + ns * 128, :].rearrange("(j p) f -> p j f", p=128), [], [rsn_t], "rsn")
                for j, t in enumerate(tl):
                    xt = xt_s[j % 2]
                    hb = hb_s[j % 2]
                    dma("sp", xt.ap, t["x"], [], [xt], "xt%d" % (j % 2))
                    norm_mod(xt, junk, ss_s[j % 2], rs_s[j % 2], Ab[t["c"]], shb[t["c"]], hb)
                    transpose16(hb, hT, j * 128, (0, 1), hTtok[j])
                anyq = any(t["q"] for t in tl)
                for ch in range(12):
                    if ch < 4 and not anyq:
                        continue
                    w_t = wq_s[wit % 2]
                    dma("pool", w_t.ap, wview(wqkv[:, ch * 512:(ch + 1) * 512]), [], [w_t], "wq%d" % (wit % 2))
                    wit += 1
                    kind = "q" if ch < 4 else ("k" if ch < 8 else "v")
                    if kind in KASKIP:
                        continue
                    cc = ch % 4
                    st = stg[ch % 2]
                    used = []
                    for j, t in enumerate(tl):
                        if kind == "q" and not t["q"]:
                            continue
                        used.append(j)
                        pb = 2 + (eit % 3)
                        sl = eit % 2
                        eit += 1
                        insts = [("matmul", dict(out=psf[pb].ap, lhsT=hT.ap[:, kc, j * 128:(j + 1) * 128], rhs=w_t.ap[:, kc, :], start=(kc == 0), stop=(kc == 15))) for kc in range(16)]
                        G("pe", insts, [hTtok[j][0], hTtok[j][1], w_t], [psf[pb]])
                        P = psf[pb]
                        if kind == "v":
                            if t["kind"] == "p":
                                r0 = t["idx"] * 128
                                V("dve", "tensor_copy", [P], [vsf[sl]], out=vsf[sl].ap, in_=P.ap)
                                dma("sp", sv[r0:r0 + 128, cc * 512:(cc + 1) * 512], vsf[sl].ap, [vsf[sl]], [], "vsf%d" % sl)
                                V("act", "copy", [P], [vst[sl]], out=vst[sl].ap, in_=P.ap)
                                dma("sp", vp[r0:r0 + 128, cc * 512:(cc + 1) * 512], vst[sl].ap, [vst[sl]], [], "vst%d" % sl)
                            else:
                                V("act", "copy", [P], [vst[sl]], out=vst[sl].ap, in_=P.ap)
                                r0 = PAST + t["idx"] * 128
                                dma("sp", vall[r0:r0 + 128, cc * 512:(cc + 1) * 512], vst[sl].ap, [vst[sl]], [], "vst%d" % sl)
                            continue
                        g_t = gq if kind == "q" else gk
                        g4 = lambda a: a.rearrange("p (g d) -> p g d", g=4)
                        V("act", "activation", [P], [e1[sl]], out=e1[sl].ap, in_=P.ap, func=AF.Square)
                        V("dve", "tensor_reduce", [e1[sl]], [ess[sl]], out=ess[sl].ap, in_=g4(e1[sl].ap), axis=AX.X, op=ALU.add)
                        rstd_from_ss(ess[sl], ess[sl], 1.0 / HD)
                        V("dve", "tensor_tensor", [P, ess[sl]], [e2[sl]], out=g4(e2[sl].ap), in0=g4(P.ap), in1=ess[sl].ap.unsqueeze(2).to_broadcast([128, 4, HD]), op=ALU.mult)
                        V("dve", "tensor_tensor", [e2[sl], g_t], [e2[sl]], out=g4(e2[sl].ap), in0=g4(e2[sl].ap), in1=g_t.ap.unsqueeze(1).to_broadcast([128, 4, HD]), op=ALU.mult)
                        if t["kind"] == "p" or "rope" in KASKIP:
                            if kind == "k" and t["kind"] == "p":
                                r0 = t["idx"] * 128
                                dma("sp", sk[r0:r0 + 128, cc * 512:(cc + 1) * 512], e2[sl].ap, [e2[sl]], [], "e2_%d" % sl)
                            V("dve", "tensor_copy", [e2[sl]], [e4[sl]], out=e4[sl].ap, in_=e2[sl].ap)
                        else:
                            Cb = rc_t.ap[:, j, :]
                            Sb = rsn_t.ap[:, j, :].rearrange("p (a j f) -> p a j f", a=2, j=2)
                            x5 = e2[sl].ap.rearrange("p (g a j f) -> p g a j f", g=4, a=2, j=2)
                            t5 = e3[sl].ap.rearrange("p (g a j f) -> p g a j f", g=4, a=2, j=2)
                            V("dve", "tensor_tensor", [e2[sl], rc_t], [e1[sl]], out=g4(e1[sl].ap), in0=g4(e2[sl].ap), in1=Cb.unsqueeze(1).to_broadcast([128, 4, HD]), op=ALU.mult)
                            V("dve", "tensor_tensor", [e2[sl], rsn_t], [e3[sl]], out=t5[:, :, :, 0, :], in0=x5[:, :, :, 1, :], in1=Sb[:, :, 0, :].unsqueeze(1).to_broadcast([128, 4, 2, 32]), op=ALU.mult)
                            V("dve", "tensor_tensor", [e2[sl], rsn_t], [e3[sl]], out=t5[:, :, :, 1, :], in0=x5[:, :, :, 0, :], in1=Sb[:, :, 1, :].unsqueeze(1).to_broadcast([128, 4, 2, 32]), op=ALU.mult)
                            V("dve", "tensor_tensor", [e1[sl], e3[sl]], [e4[sl]], out=e4[sl].ap, in0=e1[sl].ap, in1=e3[sl].ap, op=ALU.add)
                        tpb = 5 + (tit % 2)
                        tit += 1
                        insts = [("transpose", dict(out=ps16(tpb)[:, g * 128:(g + 1) * 128], in_=e4[sl].ap[:, g * 128:(g + 1) * 128], identity=idb.ap)) for g in range(4)]
                        G("pe", insts, [e4[sl], idb], [psf[tpb]])
                        V("act", "copy", [psf[tpb]], [st], out=st.ap[:, :, j * 128:(j + 1) * 128], in_=ps16(tpb)[:, 0:512].rearrange("p (g t) -> p g t", g=4))
                    if kind == "v" or not used:
                        continue
                    j_p = [j for j in used if tl[j]["kind"] == "p"]
                    j_s = [j for j in used if tl[j]["kind"] == "s"]
                    gsl = slice(cc * 4, cc * 4 + 4)
                    if j_p:
                        dst = (qTp if kind == "q" else kTp)
                        c0 = tl[j_p[0]]["idx"] * 128
                        n = len(j_p) * 128
                        dma("sp", dst[gsl, :, c0:c0 + n].rearrange("g d t -> d g t"), st.ap[:, :, j_p[0] * 128:j_p[0] * 128 + n], [st], [], "stg%dp" % (ch % 2))
                    if j_s:
                        i0 = tl[j_s[0]]["idx"]
                        n = len(j_s) * 128
                        if kind == "q":
                            dstap = qT[gsl, :, i0 * 128:i0 * 128 + n]
                        else:
                            dstap = kT[gsl, :, PAST + i0 * 128:PAST + i0 * 128 + n]
                        dma("sp", dstap.rearrange("g d t -> d g t"), st.ap[:, :, j_s[0] * 128:j_s[0] * 128 + n], [st], [], "stg%ds" % (ch % 2))
            S.barrier()

        def phaseB():
            A.reset(base0)
            KT = [A.alloc("KT%d" % i, [128, 2, NKEY], BF16) for i in range(2)]
            VA = [A.alloc("VA%d" % i, [128, 36, 257], BF16) for i in range(2)]
            QT = [A.alloc("QT%d" % i, [128, 2, NSQ * 128], BF16) for i in range(2)]
            KTP = [A.alloc("KTP%d" % i, [128, 2, 512], BF16) for i in range(2)]
            VAP = [A.alloc("VAP%d" % i, [128, 4, 257], BF16) for i in range(2)]
            QTP = [A.alloc("QTP%d" % i, [128, 2, 512], BF16) for i in range(2)]
            PT = [A.alloc("PT%d" % i, [128, 512], BF16) for i in range(3)]
            o0 = [A.alloc("o0_%d" % i, [128, 256], F32) for i in range(4)]
            of = [A.alloc("of%d" % i, [128, 256], F32) for i in range(2)]
            ob = [A.alloc("ob%d" % i, [128, 256], BF16) for i in range(2)]
            osq = [A.alloc("osq%d" % i, [128, 256], F32) for i in range(2)]
            r0t = [A.alloc("r0t%d" % i, [128, 1], F32) for i in range(4)]
            r1t = [A.alloc("r1t%d" % i, [128, 1], F32) for i in range(2)]
            oss = [A.alloc("oss%d" % i, [128, 1], F32) for i in range(2)]
            lpt = A.alloc("lpt", [128, 512], F32)
            lj = A.alloc("lj", [128, 128], F32)
            lsum = A.alloc("lsum", [128, 2], F32)
            nlam = A.alloc("nlam", [128, 1], F32)
            sgb = A.alloc("sgb", [128, 256], F32)
            load_bcast(lpt, lamp, "lpt")
            load_bcast(sgb, subg, "sgb")
            V("dve", "tensor_scalar_mul", [sgb], [sgb], out=sgb.ap, in0=sgb.ap, scalar1=(1.0 - LAMBDA_INIT))
            for q in range(2):
                V("dve", "tensor_tensor", [lpt], [lj], out=lj.ap, in0=lpt.ap[:, (2 * q) * 128:(2 * q + 1) * 128], in1=lpt.ap[:, (2 * q + 1) * 128:(2 * q + 2) * 128], op=ALU.mult)
                V("dve", "tensor_reduce", [lj], [lsum], out=lsum.ap[:, q:q + 1], in_=lj.ap, axis=AX.X, op=ALU.add)
            V("act", "activation", [lsum], [lsum], out=lsum.ap, in_=lsum.ap, func=AF.Exp)
            V("dve", "tensor_tensor", [lsum], [nlam], out=nlam.ap, in0=lsum.ap[:, 1:2], in1=lsum.ap[:, 0:1], op=ALU.subtract)
            V("dve", "tensor_scalar_add", [nlam], [nlam], out=nlam.ap, in0=nlam.ap, scalar1=-LAMBDA_INIT)
            for i in range(2):
                V("dve", "memset", [], [VA[i]], ap=VA[i].ap[:, :, 256:257], constant=1.0)
                V("dve", "memset", [], [VAP[i]], ap=VAP[i].ap[:, :, 256:257], constant=1.0)
            SCALE = HD ** -0.5
            uctr = [0]
            octr = [0]

            def attention(Kt, Vt, Qt, kcol0, nkc, vch0, qcol0, ntile, orow0, h):
                units = [(c, kc) for c in range(2) for kc in range(nkc)]
                n = len(units)
                pend = []
                nq = ntile * 128
                for idx in range(n + 2):
                    if idx < n:
                        c, kc = units[idx]
                        u = uctr[0]
                        uctr[0] += 1
                        sb_ = 4 + (u % 3)
                        pt = PT[u % 3]
                        V("pe", "matmul", [Kt, Qt], [psf[sb_]], out=psf[sb_].ap[:, 0:nq], lhsT=Kt.ap[:, c, kcol0 + kc * 128:kcol0 + (kc + 1) * 128], rhs=Qt.ap[:, c, qcol0:qcol0 + nq], start=True, stop=True)
                        V("act", "activation", [psf[sb_]], [pt], out=pt.ap[:, 0:nq], in_=psf[sb_].ap[:, 0:nq], func=AF.Exp, scale=SCALE)
                        pend.append((c, kc, pt))
                    if idx >= 2:
                        c, kc, pt = pend[idx - 2]
                        for i in range(ntile):
                            V("pe", "matmul", [pt, Vt], [psf[i]], out=psf[i].ap[:, 0:257], lhsT=pt.ap[:, i * 128:(i + 1) * 128], rhs=Vt.ap[:, vch0 + kc, :], start=(kc == 0), stop=(kc == nkc - 1))
                        if kc != nkc - 1:
                            continue
                        for i in range(ntile):
                            acc = psf[i]
                            if c == 0:
                                V("dve", "reciprocal", [acc], [r0t[i]], out=r0t[i].ap, in_=acc.ap[:, 256:257])
                                V("dve", "tensor_scalar_mul", [acc, r0t[i]], [o0[i]], out=o0[i].ap, in0=acc.ap[:, 0:256], scalar1=r0t[i].ap)
                            else:
                                k = octr[0] % 2
                                octr[0] += 1
                                V("dve", "reciprocal", [acc], [r1t[k]], out=r1t[k].ap, in_=acc.ap[:, 256:257])
                                V("dve", "tensor_tensor", [r1t[k], nlam], [r1t[k]], out=r1t[k].ap, in0=r1t[k].ap, in1=nlam.ap, op=ALU.mult)
                                V("dve", "scalar_tensor_tensor", [acc, r1t[k], o0[i]], [of[k]], out=of[k].ap, in0=acc.ap[:, 0:256], scalar=r1t[k].ap, in1=o0[i].ap, op0=ALU.mult, op1=ALU.add)
                                V("dve", "tensor_tensor", [of[k]], [osq[k]], out=osq[k].ap, in0=of[k].ap, in1=of[k].ap, op=ALU.mult)
                                V("dve", "tensor_reduce", [osq[k]], [oss[k]], out=oss[k].ap, in_=osq[k].ap, axis=AX.X, op=ALU.add)
                                V("dve", "tensor_scalar", [oss[k]], [oss[k]], out=oss[k].ap, in0=oss[k].ap, scalar1=1.0 / 256, scalar2=EPS, op0=ALU.mult, op1=ALU.add)
                                V("act", "activation", [oss[k]], [oss[k]], out=oss[k].ap, in_=oss[k].ap, func=AF.Ln)
                                V("act", "activation", [oss[k]], [oss[k]], out=oss[k].ap, in_=oss[k].ap, func=AF.Exp, scale=-0.5)
                                V("dve", "scalar_tensor_tensor", [of[k], oss[k], sgb], [ob[k]], out=ob[k].ap, in0=of[k].ap, scalar=oss[k].ap, in1=sgb.ap, op0=ALU.mult, op1=ALU.mult)
                                r = orow0 + i * 128
                                dma("sp", oall[r:r + 128, h * 256:(h + 1) * 256], ob[k].ap, [ob[k]], [], "ob%d" % k)

            for h in range(NH):
                sl = h % 2
                dma("sp", KT[sl].ap, kT[2 * h:2 * h + 2, :, :].rearrange("c d t -> d c t"), [], [KT[sl]], "KT%d" % sl)
                dma("sp", QT[sl].ap, qT[2 * h:2 * h + 2, :, :].rearrange("c d t -> d c t"), [], [QT[sl]], "QT%d" % sl)
                dma("sp", VA[sl].ap[:, :, 0:256], vall[:, h * 256:(h + 1) * 256].rearrange("(k p) e -> p k e", p=128), [], [VA[sl]], "VA%d" % sl)
                dma("sp", KTP[sl].ap, kTp[2 * h:2 * h + 2, :, :].rearrange("c d t -> d c t"), [], [KTP[sl]], "KTP%d" % sl)
                dma("sp", QTP[sl].ap, qTp[2 * h:2 * h + 2, :, :].rearrange("c d t -> d c t"), [], [QTP[sl]], "QTP%d" % sl)
                dma("sp", VAP[sl].ap[:, :, 0:256], vp[:, h * 256:(h + 1) * 256].rearrange("(k p) e -> p k e", p=128), [], [VAP[sl]], "VAP%d" % sl)
                for sq in range(2):
                    attention(KTP[sl], VAP[sl], QTP[sl], sq * 256, 2, sq * 2, sq * 256, 2, sq * 256, h)
                for qb in range(0, NSQ, 4):
                    nt = min(4, NSQ - qb)
                    attention(KT[sl], VA[sl], QT[sl], 0, 36, 0, qb * 128, nt, 512 + qb * 128, h)
            S.barrier()

        def phaseC():
            A.reset(base0)
            wob = A.alloc("wob", [128, 16, D], BF16)
            g1b = [A.alloc("g1b%d" % c, [128, D], F32) for c in range(2)]
            ot_s = [A.alloc("ot%d" % i, [128, D], BF16) for i in range(2)]
            oT_s = [A.alloc("oT%d" % i, [128, 16, 128], BF16) for i in range(2)]
            xt_s = [A.alloc("xt%d" % i, [128, D], F32) for i in range(2)]
            tm_s = [A.alloc("tm%d" % i, [128, 512], F32) for i in range(2)]
            wobq = [T(None, "wobq%d" % q) for q in range(4)]
            for q in range(4):
                dma("pool", wob.ap[:, :, q * 512:(q + 1) * 512], wview(wo[:, q * 512:(q + 1) * 512]), [], [wobq[q]], "wob%d" % q)
            for c in range(2):
                load_bcast(g1b[c], modrow(0, c, 2), "g1b%d" % c)
            ectr = 0
            for i in range(21):
                sl = i % 2
                c = 0 if i < 4 else 1
                xt = xt_s[sl]
                dma("sp", ot_s[sl].ap, oall[i * 128:(i + 1) * 128, :], [], [ot_s[sl]], "ot%d" % sl)
                dma("sp", xt.ap, xin(i), [], [xt], "xt%d" % sl)
                transpose16(ot_s[sl], oT_s[sl], 0, (0, 1), [oT_s[sl], oT_s[sl]])
                for q in range(4):
                    pb = 2 + (ectr % 3)
                    tm = tm_s[ectr % 2]
                    ectr += 1
                    insts = [("matmul", dict(out=psf[pb].ap, lhsT=oT_s[sl].ap[:, kc, :], rhs=wob.ap[:, kc, q * 512:(q + 1) * 512], start=(kc == 0), stop=(kc == 15))) for kc in range(16)]
                    G("pe", insts, [oT_s[sl], wobq[q]], [psf[pb]])
                    V("dve", "tensor_tensor", [psf[pb], g1b[c]], [tm], out=tm.ap, in0=psf[pb].ap, in1=g1b[c].ap[:, q * 512:(q + 1) * 512], op=ALU.mult)
                    V("dve", "tensor_tensor", [tm, xt], [xt], out=xt.ap[:, q * 512:(q + 1) * 512], in0=xt.ap[:, q * 512:(q + 1) * 512], in1=tm.ap, op=ALU.add)
                dma("sp", resid(i), xt.ap, [xt], [], "xo%d" % sl)
            S.barrier()

        def ffn_phase(L, blocks, experts, use_comb):
            A.reset(base0)
            Ab2 = A.alloc("A2b", [128, D], F32)
            sh2 = A.alloc("sh2b", [128, D], F32)
            xt_s = [A.alloc("xt%d" % i, [128, D], F32) for i in range(2)]
            junk = A.alloc("junk", [128, D], BF16)
            hb_s = [A.alloc("hb%d" % i, [128, D], BF16) for i in range(2)]
            ss_s = [A.alloc("ss%d" % i, [128, 1], F32) for i in range(2)]
            rs_s = [A.alloc("rs%d" % i, [128, 1], F32) for i in range(2)]
            MT = 8
            h2T = A.alloc("h2T", [128, 16, MT * 128], BF16)
            h2tok = [[T(None, "h2T_%d_%d" % (j, q)) for q in range(2)] for j in range(MT)]
            hidT = A.alloc("hidT", [128, 22, MT * 128], BF16)
            wg_s = [A.alloc("wg%d" % i, [128, 16, 256], BF16) for i in range(2)]
            wu_s = [A.alloc("wu%d" % i, [128, 16, 256], BF16) for i in range(2)]
            wd_s = [A.alloc("wd%d" % i, [128, 22, 256], BF16) for i in range(2)]
            sil = [A.alloc("sil%d" % i, [128, 512], F32) for i in range(2)]
            ct_s = [A.alloc("ct%d" % i, [128, 256], F32) for i in range(4)]
            cmb = A.alloc("cmb", [128, MT, NE], F32)
            ytok = {}

            gu_jobs = []
            d_jobs = []
            for bi in range(len(blocks)):
                for ex in experts:
                    for cb_ in range(11):
                        gu_jobs.append((ex, cb_))
                    for q8 in range(8):
                        d_jobs.append((ex, q8))
            gu_issued = [0]
            d_issued = [0]

            def issue_gu(upto):
                while gu_issued[0] <= upto and gu_issued[0] < len(gu_jobs):
                    k = gu_issued[0]
                    ex, cb_ = gu_jobs[k]
                    dma("pool", wg_s[k % 2].ap, wview(ex["g"](cb_ * 256, 256)), [], [wg_s[k % 2]], "wg%d" % (k % 2))
                    dma("pool", wu_s[k % 2].ap, wview(ex["u"](cb_ * 256, 256)), [], [wu_s[k % 2]], "wu%d" % (k % 2))
                    gu_issued[0] += 1

            def issue_d(upto):
                while d_issued[0] <= upto and d_issued[0] < len(d_jobs):
                    k = d_issued[0]
                    ex, q8 = d_jobs[k]
                    dma("pool", wd_s[k % 2].ap, wview(ex["d"](q8 * 256, 256)), [], [wd_s[k % 2]], "wd%d" % (k % 2))
                    d_issued[0] += 1

            gk_ = 0
            dk_ = 0
            sctr = 0
            gctr = 0
            for (c, tiles) in blocks:
                nt = len(tiles)
                ntok = nt * 128
                subs = []
                o_ = 0
                while o_ < ntok:
                    n_ = min(512, ntok - o_)
                    subs.append((o_, n_))
                    o_ += n_
                ns_ = len(subs)
                make_Ab(Ab2, xt_s[1], modrow(L, c, 4), n2g[L:L + 1, :], "A2b")
                load_bcast(sh2, modrow(L, c, 3), "sh2b")
                if use_comb:
                    r0 = tiles[0] * 128
                    dma("sp", cmb.ap[:, 0:nt, :], combd[r0:r0 + ntok, :].rearrange("(j p) e -> p j e", p=128), [], [cmb], "cmb")
                for j, ti in enumerate(tiles):
                    xt = xt_s[j % 2]
                    dma("sp", xt.ap, resid(ti), [], [xt], "xt%d" % (j % 2))
                    norm_mod(xt, junk, ss_s[j % 2], rs_s[j % 2], Ab2, sh2, hb_s[j % 2])
                    transpose16(hb_s[j % 2], h2T, j * 128, (6, 7), h2tok[j])
                load_bcast(Ab2, modrow(L, c, 5), "g2b")
                h2reads = [b for j in range(nt) for b in h2tok[j]]
                for ei, ex in enumerate(experts):
                    issue_d(dk_)
                    for cb_ in range(11):
                        issue_gu(gk_ + 1)
                        wg = wg_s[gk_ % 2]
                        wu = wu_s[gk_ % 2]
                        gk_ += 1
                        for half in range(2):
                            jf = cb_ * 2 + half
                            for (wt, base) in ((wg, 0), (wu, 3)):
                                insts = []
                                for kc in range(16):
                                    for si, (o_, n_) in enumerate(subs):
                                        insts.append(("matmul", dict(out=psf[base + si].ap[:, 0:n_], lhsT=wt.ap[:, kc, half * 128:(half + 1) * 128], rhs=h2T.ap[:, kc, o_:o_ + n_], start=(kc == 0), stop=(kc == 15))))
                                G("pe", insts, h2reads + [wt], [psf[base + si] for si in range(ns_)])
                            for si, (o_, n_) in enumerate(subs):
                                st = sil[sctr % 2]
                                sctr += 1
                                V("act", "activation", [psf[si]], [st], out=st.ap[:, 0:n_], in_=psf[si].ap[:, 0:n_], func=AF.Silu)
                                V("dve", "tensor_tensor", [st, psf[3 + si]], [hidT], out=hidT.ap[:, jf, o_:o_ + n_], in0=st.ap[:, 0:n_], in1=psf[3 + si].ap[:, 0:n_], op=ALU.mult)
                    for q8 in range(8):
                        issue_d(dk_ + 1)
                        wd = wd_s[dk_ % 2]
                        dk_ += 1
                        for j, ti in enumerate(tiles):
                            pb = 6 + (gctr % 2)
                            ct = ct_s[gctr % 4]
                            ckey = "ct%d" % (gctr % 4)
                            gctr += 1
                            insts = [("matmul", dict(out=psf[pb].ap[:, 0:256], lhsT=hidT.ap[:, f, j * 128:(j + 1) * 128], rhs=wd.ap[:, f, :], start=(f == 0), stop=(f == 21))) for f in range(22)]
                            G("pe", insts, [hidT, wd], [psf[pb]])
                            if use_comb:
                                V("dve", "scalar_tensor_tensor", [psf[pb], cmb, Ab2], [ct], out=ct.ap, in0=psf[pb].ap[:, 0:256], scalar=cmb.ap[:, j, ei:ei + 1], in1=Ab2.ap[:, q8 * 256:(q8 + 1) * 256], op0=ALU.mult, op1=ALU.mult)
                            else:
                                V("dve", "tensor_tensor", [psf[pb], Ab2], [ct], out=ct.ap, in0=psf[pb].ap[:, 0:256], in1=Ab2.ap[:, q8 * 256:(q8 + 1) * 256], op=ALU.mult)
                            yk = ytok.setdefault((ti, q8), T(None, "y_%d_%d" % (ti, q8)))
                            dma("pool", resid(ti)[:, q8 * 256:(q8 + 1) * 256], ct.ap, [ct], [yk], ckey, accum_op=ALU.add)
            S.barrier()

        def dense_expert(half):
            return dict(
                g=lambda c0, n, half=half: wgu[:, half * DFE + c0:half * DFE + c0 + n],
                u=lambda c0, n, half=half: wgu[:, DFF + half * DFE + c0:DFF + half * DFE + c0 + n],
                d=lambda c0, n, half=half: wdn[half * DFE:(half + 1) * DFE, c0:c0 + n],
            )

        def moe_expert(ei):
            return dict(
                g=lambda c0, n, ei=ei: mgu[ei, :, c0:c0 + n],
                u=lambda c0, n, ei=ei: mgu[ei, :, DFE + c0:DFE + c0 + n],
                d=lambda c0, n, ei=ei: mdn[ei, :, c0:c0 + n],
            )

        def phaseE1():
            A.reset(base0)
            Ab = A.alloc("A1b", [128, D], F32)
            shb1 = A.alloc("sh1b", [128, D], F32)
            xt_s = [A.alloc("xt%d" % i, [128, D], F32) for i in range(2)]
            junk = A.alloc("junk", [128, D], BF16)
            hb_s = [A.alloc("hb%d" % i, [128, D], BF16) for i in range(2)]
            ss_s = [A.alloc("ss%d" % i, [128, 1], F32) for i in range(2)]
            rs_s = [A.alloc("rs%d" % i, [128, 1], F32) for i in range(2)]
            for c, tiles in ((0, [0, 1, 2, 3]), (1, list(range(4, 21)))):
                make_Ab(Ab, xt_s[1], modrow(1, c, 1), n1g[1:2, :], "A1b")
                load_bcast(shb1, modrow(1, c, 0), "sh1b")
                for j, ti in enumerate(tiles):
                    xt = xt_s[j % 2]
                    dma("sp", xt.ap, resid(ti), [], [xt], "xt%d" % (j % 2))
                    norm_mod(xt, junk, ss_s[j % 2], rs_s[j % 2], Ab, shb1, hb_s[j % 2])
                    dma("sp", h1all[ti * 128:(ti + 1) * 128, :], hb_s[j % 2].ap, [hb_s[j % 2]], [], "h1o%d" % (j % 2))
            S.barrier()

        def phaseE2():
            A.reset(base0)
            pwb = A.alloc("pwb", [128, 4, 4, 512], BF16)
            wrb = A.alloc("wrb", [128, NE, D], F32)
            brb = A.alloc("brb", [128, NE], F32)
            psg = A.alloc("psg", [128, D], F32)
            Ab2 = A.alloc("A2b", [128, D], F32)
            sh2 = A.alloc("sh2b", [128, D], F32)
            h3_s = [A.alloc("h3_%d" % i, [128, 3, D], BF16) for i in range(2)]
            ap_s = [A.alloc("ap%d" % i, [128, 3, 4, 128], BF16) for i in range(2)]
            xt_s = [A.alloc("xt%d" % i, [128, D], F32) for i in range(2)]
            junk = A.alloc("junk", [128, D], BF16)
            rtmp = A.alloc("rtmp", [128, D], F32)
            pT = A.alloc("pT", [128, 16, 128], BF16)
            tm_s = [A.alloc("tm%d" % i, [128, 512], F32) for i in range(2)]
            ss_s = [A.alloc("ss%d" % i, [128, 1], F32) for i in range(2)]
            rs_s = [A.alloc("rs%d" % i, [128, 1], F32) for i in range(2)]
            lg = [A.alloc("lg%d" % i, [128, NE], F32) for i in range(2)]
            l2 = [A.alloc("l2_%d" % i, [128, NE], F32) for i in range(2)]
            mk1 = [A.alloc("mk1_%d" % i, [128, NE], F32) for i in range(2)]
            mk2 = [A.alloc("mk2_%d" % i, [128, NE], F32) for i in range(2)]
            m1 = [A.alloc("m1_%d" % i, [128, 1], F32) for i in range(2)]
            m2 = [A.alloc("m2_%d" % i, [128, 1], F32) for i in range(2)]
            gt = [A.alloc("gt%d" % i, [128, 2], F32) for i in range(2)]
            cmo = [A.alloc("cmo%d" % i, [128, NE], F32) for i in range(2)]
            for g in range(4):
                dma("pool", pwb.ap[:, g, :, :], wview(poolw[g]), [], [pwb], "pwb")
            dma("sp", wrb.ap.rearrange("p e d -> p (e d)"), wrT.rearrange("(o e) d -> o (e d)", o=1).partition_broadcast(128), [], [wrb], "wrb")
            load_bcast(brb, br, "brb")

            def prev_next(ti):
                if ti < 4:
                    base = (ti // 2) * 2
                    return (base, ti, base + 1)
                i = ti - 4
                p = 20 if i == 0 else ti - 1
                n = 20 if i == 15 else ti + 1
                return (p, ti, n)

            ectr = 0
            for c, tiles in ((0, [0, 1, 2, 3]), (1, list(range(4, 20)))):
                load_bcast(psg, pools, "psg")
                load_bcast(sh2, modrow(1, c, 2), "psg1")
                V("dve", "tensor_tensor", [psg, sh2], [psg], out=psg.ap, in0=psg.ap, in1=sh2.ap, op=ALU.mult)
                make_Ab(Ab2, rtmp, modrow(1, c, 4), n2g[1:2, :], "A2b")
                load_bcast(sh2, modrow(1, c, 3), "sh2b")
                for j, ti in enumerate(tiles):
                    sl = j % 2
                    h3 = h3_s[sl]
                    apt = ap_s[sl]
                    xt = xt_s[sl]
                    pr = prev_next(ti)
                    for s3 in range(3):
                        dma("sp", h3.ap[:, s3, :], h1all[pr[s3] * 128:(pr[s3] + 1) * 128, :], [], [h3], "h3_%d_%d" % (sl, s3))
                    dma("pool", apt.ap, apool[ti].rearrange("p (s g t) -> p s g t", s=3, g=4), [], [apt], "ap%d" % sl)
                    dma("sp", xt.ap, resid(ti), [], [xt], "xt%d" % sl)
                    for g in range(4):
                        insts = []
                        for cc in range(4):
                            for s3 in range(3):
                                insts.append(("matmul", dict(out=psf[g].ap[:, cc * 128:(cc + 1) * 128], lhsT=h3.ap[:, s3, g * 512 + cc * 128:g * 512 + (cc + 1) * 128], rhs=apt.ap[:, s3, g, :], start=(s3 == 0), stop=(s3 == 2))))
                        G("pe", insts, [h3, apt], [psf[g]])
                        if g % 2 == 0:
                            V("dve", "tensor_copy", [psf[g]], [pT], out=pT.ap[:, g * 4:(g + 1) * 4, :], in_=psf[g].ap.rearrange("p (k t) -> p k t", k=4))
                        else:
                            V("act", "copy", [psf[g]], [pT], out=pT.ap[:, g * 4:(g + 1) * 4, :], in_=psf[g].ap.rearrange("p (k t) -> p k t", k=4))
                    for g in range(4):
                        pb = 4 + (ectr % 3)
                        tm = tm_s[ectr % 2]
                        ectr += 1
                        insts = [("matmul", dict(out=psf[pb].ap, lhsT=pT.ap[:, g * 4 + cc, :], rhs=pwb.ap[:, g, cc, :], start=(cc == 0), stop=(cc == 3))) for cc in range(4)]
                        G("pe", insts, [pT, pwb], [psf[pb]])
                        V("dve", "tensor_tensor", [psf[pb], psg], [tm], out=tm.ap, in0=psf[pb].ap, in1=psg.ap[:, g * 512:(g + 1) * 512], op=ALU.mult)
                        V("dve", "tensor_tensor", [tm, xt], [xt], out=xt.ap[:, g * 512:(g + 1) * 512], in0=xt.ap[:, g * 512:(g + 1) * 512], in1=tm.ap, op=ALU.add)
                    dma("sp", resid(ti), xt.ap, [xt], [], "xo%d" % sl)
                    V("act", "activation", [xt], [junk, ss_s[sl]], out=junk.ap, in_=xt.ap, func=AF.Square, accum_out=ss_s[sl].ap)
                    rstd_from_ss(ss_s[sl], rs_s[sl], 1.0 / D)
                    V("dve", "scalar_tensor_tensor", [xt, rs_s[sl], Ab2], [xt], out=xt.ap, in0=xt.ap, scalar=rs_s[sl].ap, in1=Ab2.ap, op0=ALU.mult, op1=ALU.mult)
                    V("dve", "tensor_tensor", [xt, sh2], [xt], out=xt.ap, in0=xt.ap, in1=sh2.ap, op=ALU.add)
                    for ei in range(NE):
                        V("dve", "tensor_tensor", [xt, wrb], [rtmp], out=rtmp.ap, in0=xt.ap, in1=wrb.ap[:, ei, :], op=ALU.mult)
                        V("dve", "tensor_reduce", [rtmp], [lg[sl]], out=lg[sl].ap[:, ei:ei + 1], in_=rtmp.ap, axis=AX.X, op=ALU.add)
                    V("dve", "tensor_tensor", [lg[sl], brb], [lg[sl]], out=lg[sl].ap, in0=lg[sl].ap, in1=brb.ap, op=ALU.add)
                    V("dve", "tensor_reduce", [lg[sl]], [m1[sl]], out=m1[sl].ap, in_=lg[sl].ap, axis=AX.X, op=ALU.max)
                    V("dve", "tensor_single_scalar", [lg[sl], m1[sl]], [mk1[sl]], out=mk1[sl].ap, in_=lg[sl].ap, scalar=m1[sl].ap, op=ALU.is_equal)
                    V("dve", "scalar_tensor_tensor", [mk1[sl], lg[sl]], [l2[sl]], out=l2[sl].ap, in0=mk1[sl].ap, scalar=-1e30, in1=lg[sl].ap, op0=ALU.mult, op1=ALU.add)
                    V("dve", "tensor_reduce", [l2[sl]], [m2[sl]], out=m2[sl].ap, in_=l2[sl].ap, axis=AX.X, op=ALU.max)
                    V("dve", "tensor_single_scalar", [l2[sl], m2[sl]], [mk2[sl]], out=mk2[sl].ap, in_=l2[sl].ap, scalar=m2[sl].ap, op=ALU.is_equal)
                    V("dve", "tensor_tensor", [m1[sl], m2[sl]], [gt[sl]], out=gt[sl].ap[:, 0:1], in0=m1[sl].ap, in1=m2[sl].ap, op=ALU.subtract)
                    V("dve", "tensor_tensor", [m1[sl], m2[sl], gt[sl]], [gt[sl]], out=gt[sl].ap[:, 1:2], in0=m2[sl].ap, in1=m1[sl].ap, op=ALU.subtract)
                    V("act", "activation", [gt[sl]], [gt[sl]], out=gt[sl].ap, in_=gt[sl].ap, func=AF.Sigmoid)
                    V("dve", "tensor_scalar_mul", [mk1[sl], gt[sl]], [cmo[sl]], out=cmo[sl].ap, in0=mk1[sl].ap, scalar1=gt[sl].ap[:, 0:1])
                    V("dve", "scalar_tensor_tensor", [mk2[sl], gt[sl], cmo[sl]], [cmo[sl]], out=cmo[sl].ap, in0=mk2[sl].ap, scalar=gt[sl].ap[:, 1:2], in1=cmo[sl].ap, op0=ALU.mult, op1=ALU.add)
                    dma("sp", combd[ti * 128:(ti + 1) * 128, :], cmo[sl].ap, [cmo[sl]], [], "cmo%d" % sl)
            S.barrier()

        if not KSKIP0:
            phase0()
        if MAXPH >= 1:
            phaseA()
        if MAXPH >= 2:
            phaseB()
        if MAXPH >= 3:
            phaseC()
        if MAXPH >= 4:
            ffn_phase(0, [(0, [0, 1, 2, 3]), (1, list(range(4, 10))), (1, list(range(10, 16))), (1, list(range(16, 21)))],
                      [dense_expert(0), dense_expert(1)], False)
        if MAXPH >= 5:
            phaseE1()
        if MAXPH >= 6:
            phaseE2()
        if MAXPH >= 7:
            ffn_phase(1, [(0, [0, 1, 2, 3]), (1, list(range(4, 12))), (1, list(range(12, 20)))],
                      [moe_expert(e) for e in range(NE)], True)
        S.emit()
    return nc


def _bf16_round(a):
    return a


_PROG = {}


def kernel(x_prompt, x_sample, c, cache_k, cache_v, c_ctx, ada_w, ada_b, norm1_g, norm2_g,
           attn_w_qkv, attn_w_o, attn_q_norm, attn_k_norm, attn_lambda, attn_subln_g,
           pool_w, pool_scale, ffn_w_gu, ffn_w_down,
           moe_w_router, moe_b_router, moe_w_gu, moe_w_down):
    f = lambda a: np.ascontiguousarray(np.asarray(a, dtype=np.float32))
    x_prompt, x_sample, c, cache_k, cache_v, c_ctx = map(f, (x_prompt, x_sample, c, cache_k, cache_v, c_ctx))
    if "nc" not in _PROG:
        _PROG["nc"] = build_program()
    nc = _PROG["nc"]

    shared = dict(
        ada_w=f(ada_w), ada_b=f(ada_b), n1g=f(norm1_g), n2g=f(norm2_g),
        wqkv=f(attn_w_qkv).reshape(D, 3 * D), wo=f(attn_w_o).reshape(D, D),
        qng=f(attn_q_norm).reshape(1, HD), kng=f(attn_k_norm).reshape(1, HD),
        lamp=f(attn_lambda).reshape(1, 4 * HD), subg=f(attn_subln_g).reshape(1, 256),
        poolw=f(pool_w).reshape(4, 512, 512), pools=f(pool_scale).reshape(1, D),
        wgu=f(ffn_w_gu).reshape(D, 2 * DFF), wdn=f(ffn_w_down).reshape(DFF, D),
        wrT=np.ascontiguousarray(f(moe_w_router).reshape(D, NE).T), br=f(moe_b_router).reshape(1, NE),
        mgu=f(moe_w_gu).reshape(NE, D, 2 * DFE), mdn=f(moe_w_down).reshape(NE, DFE, D),
        ident=np.eye(128, dtype=np.float32),
    )
    GRID_W = 64
    nfreq = HD // 4
    freqs = (np.float32(10000.0) ** (-np.arange(nfreq, dtype=np.float32) / np.float32(nfreq))).astype(np.float32)
    windows = (2, 4, 8, 16)

    def pool_blocks(t0, L):
        out = np.zeros((128, 3, 4, 128), np.float32)
        t = np.arange(t0, t0 + 128)
        for g, w in enumerate(windows):
            lo = np.clip(t - w // 2, 0, L - 1)
            hi = np.clip(t + w // 2 - 1, 0, L - 1)
            cnt = (hi - lo + 1).astype(np.float32)
            for blk in range(3):
                s = np.arange(t0 + (blk - 1) * 128, t0 + blk * 128)
                m = ((s[:, None] >= lo[None, :]) & (s[:, None] <= hi[None, :])).astype(np.float32) / cnt[None, :]
                m = m - (s[:, None] == t[None, :]).astype(np.float32)
                out[:, blk, g, :] = m
        return out

    in_maps = []
    for core in range(8):
        b = core // 2
        s = core % 2
        own = np.arange(s * 2048, (s + 1) * 2048)
        if s == 0:
            halo = np.arange(2048, 2176)
            others = np.arange(2176, 4096)
        else:
            halo = np.arange(1920, 2048)
            others = np.arange(0, 1920)
        order = np.concatenate([own, halo, others])
        r = (order // GRID_W).astype(np.float32)
        col = (order % GRID_W).astype(np.float32)
        ang = np.stack([r[:, None] * freqs[None, :], col[:, None] * freqs[None, :]], axis=1).astype(np.float32)
        cs = np.cos(ang).astype(np.float32)
        sn = np.sin(ang).astype(np.float32)
        rc = np.stack([cs, cs], axis=2).reshape(4096, 128)
        rsn = np.stack([-sn, sn], axis=2).reshape(4096, 128)
        cond = np.stack([c_ctx, c[b]], axis=0)
        condT = np.ascontiguousarray(cond.reshape(2, 16, 128).transpose(2, 0, 1).reshape(128, 32))
        ap = np.zeros((20, 128, 3, 4, 128), np.float32)
        for i in range(4):
            ap[i] = pool_blocks((i % 2) * 128, 256)
        for i in range(16):
            ap[4 + i] = pool_blocks(s * 2048 + i * 128, 4096)
        m = dict(shared)
        m.update(
            xp=np.ascontiguousarray(x_prompt[2 * core:2 * core + 2].reshape(512, D)),
            xs=np.ascontiguousarray(x_sample[b][order]),
            condT=condT,
            ck=np.ascontiguousarray(cache_k[b, 0].reshape(PAST, D)),
            cv=np.ascontiguousarray(cache_v[b, 0].reshape(PAST, D)),
            ropeC=np.ascontiguousarray(rc), ropeS=np.ascontiguousarray(rsn),
            apool=ap.reshape(20, 128, 3 * 4 * 128),
        )
        in_maps.append({k: v for k, v in m.items() if k in _DECL})

    res = run_bass_kernel_spmd(nc, in_maps, core_ids=list(range(8)))
    _PROG['last'] = res
    y_prompt = np.zeros((16, 256, D), np.float32)
    y_sample = np.zeros((4, 4096, D), np.float32)
    state_k = np.zeros((16, 1, 256, NH, 256), np.float32)
    state_v = np.zeros((16, 1, 256, NH, 256), np.float32)
    for core in range(8):
        r = res.results[core]
        b = core // 2
        s = core % 2
        y_prompt[2 * core:2 * core + 2] = r["yp"].reshape(2, 256, D)
        y_sample[b, s * 2048:(s + 1) * 2048] = r["ys"]
        state_k[2 * core:2 * core + 2, 0] = r["sk"].reshape(2, 256, NH, 256)
        state_v[2 * core:2 * core + 2, 0] = r["sv"].reshape(2, 256, NH, 256)
    return (y_prompt, y_sample, state_k, state_v)
```

```python
import math
import os
import numpy as np
from contextlib import ExitStack
import concourse.bass as bass
import concourse.mybir as mybir
from concourse.bass_utils import run_bass_kernel_spmd

F32 = mybir.dt.float32
BF16 = mybir.dt.bfloat16
U8 = mybir.dt.uint8
AF = mybir.ActivationFunctionType
ALU = mybir.AluOpType
AX = mybir.AxisListType

D = 2048
NH = 8
HD = 128
DFF = 5632
DFE = 2816
NE = 8
EPS = 1e-6
NPT = 4
NSO = 16
NSQ = 17
NSA = 32
PAST = 512
NKEY = PAST + NSA * 128
LAMBDA_INIT = 0.8 - 0.6 * math.exp(-0.3 * 0)
ARENA_BYTES = 206 * 1024
MAXPH = int(os.environ.get('KMAXPH', '7'))
_DECL = []
KDEBUG = [x for x in os.environ.get('KDEBUG', '').split(',') if x]
_DBG = []
KSKIP0 = bool(int(os.environ.get('KSKIP0', '0')))
KABLK = int(os.environ.get('KABLK', '99'))
KASKIP = [x for x in os.environ.get('KASKIP', '').split(',') if x]


class Buf:
    __slots__ = ("name", "w", "r")

    def __init__(self, name):
        self.name = name
        self.w = {}
        self.r = {}


class T:
    __slots__ = ("ap", "b", "name", "excl")

    def __init__(self, ap, name, excl=False):
        self.ap = ap
        self.b = Buf(name)
        self.name = name
        self.excl = excl


class Sched:
    ENGS = ("pe", "act", "dve", "pool", "sp")

    def __init__(self, nc, stack):
        self.nc = nc
        self.stack = stack
        self.ops = {e: [] for e in self.ENGS}
        self.sems = {}
        self.waited = {e: {} for e in self.ENGS}
        self.dma_map = {}
        self.nsem = 0
        for e in self.ENGS:
            self._sem("prog_" + e)

    def _sem(self, key):
        if key not in self.sems:
            h = self.stack.enter_context(self.nc.semaphore(key))
            self.sems[key] = [h, 0]
            self.nsem += 1
        return self.sems[key]

    def op(self, eng, body, reads=(), writes=(), dma=None):
        ex = [t for t in reads if t.excl]
        if ex:
            reads = [t for t in reads if not t.excl]
            writes = list(writes) + [t for t in ex if t not in writes]
        waits = {}
        for t in reads:
            for k, v in t.b.w.items():
                if waits.get(k, 0) < v:
                    waits[k] = v
        for t in writes:
            for d in (t.b.w, t.b.r):
                for k, v in d.items():
                    if waits.get(k, 0) < v:
                        waits[k] = v
        wl = []
        wd = self.waited[eng]
        for k, v in waits.items():
            if wd.get(k, 0) < v:
                wd[k] = v
                wl.append((self.sems[k][0], v))
        if dma is None:
            key = "prog_" + eng
            amt = 1
        else:
            dk = (eng, dma)
            if dk not in self.dma_map:
                n = sum(1 for k in self.dma_map if k[0] == eng)
                self.dma_map[dk] = "dq%s%d" % (eng, n)
            key = self.dma_map[dk]
            amt = 16
        s = self._sem(key)
        s[1] += amt
        ev = s[1]
        self.ops[eng].append((wl, body, s[0], amt))
        for t in reads:
            if t.b.r.get(key, 0) < ev:
                t.b.r[key] = ev
        for t in writes:
            t.b.w = {key: ev}
            t.b.r = {}

    def barrier(self):
        snap = {k: v[1] for k, v in self.sems.items() if v[1] > 0}
        for e in self.ENGS:
            wd = self.waited[e]
            wl = []
            for k, v in snap.items():
                if wd.get(k, 0) < v:
                    wd[k] = v
                    wl.append((self.sems[k][0], v))
            s = self.sems["prog_" + e]
            s[1] += 1
            self.ops[e].append((wl, (lambda eng: eng.nop()), s[0], 1))
        self.dma_map = {}

    def emit(self):
        ops = self.ops

        def replay(name):
            def f(eng):
                for wl, body, sem, amt in ops[name]:
                    for (h, v) in wl:
                        eng.wait_ge(h, v)
                    ins = body(eng)
                    ins.then_inc(sem, amt)
            return f

        with self.nc.Block() as block:
            block.tensor(replay("pe"))
            block.scalar(replay("act"))
            block.vector(replay("dve"))
            block.gpsimd(replay("pool"))
            block.sync(replay("sp"))


class Arena:
    def __init__(self, nc):
        self.t = nc.alloc_sbuf_tensor("arena", [128, ARENA_BYTES], U8)
        self.off = 0
        self.n = 0

    def reset(self, to=0):
        self.off = to

    def alloc(self, name, shape, dtype):
        esz = 4 if dtype == F32 else 2
        nbytes = int(np.prod(shape[1:])) * esz
        assert self.off + nbytes <= ARENA_BYTES, (name, self.off, nbytes)
        a = self.t[:, self.off:self.off + nbytes].bitcast(dtype)
        self.off += (nbytes + 63) // 64 * 64
        if len(shape) == 3:
            a = a.rearrange("p (a b) -> p a b", a=shape[1])
        elif len(shape) == 4:
            a = a.rearrange("p (a b c) -> p a b c", a=shape[1], b=shape[2])
        self.n += 1
        return T(a, "%s_%d" % (name, self.n))


def build_program():
    nc = bass.Bass("TRN2", target_bir_lowering=False)

    def din(name, shape, dt=F32, minph=0):
        if MAXPH < minph:
            return None
        _DECL.append(name)
        return nc.dram_tensor(name, shape, dt, kind="ExternalInput").ap()

    def dout(name, shape):
        return nc.dram_tensor(name, shape, F32, kind="ExternalOutput").ap()

    def dscr(name, shape, dt):
        if name in KDEBUG:
            _DBG.append(name)
            return nc.dram_tensor(name, shape, dt, kind="ExternalOutput").ap()
        return nc.dram_tensor(name, shape, dt, kind="Internal").ap()

    xp = din("xp", [512, D])
    xs = din("xs", [4096, D])
    condT = din("condT", [128, 32])
    ck = din("ck", [PAST, D])
    cv = din("cv", [PAST, D])
    ropeC = din("ropeC", [4096, 128])
    ropeS = din("ropeS", [4096, 128])
    ada_w = din("ada_w", [2, D, 6 * D], minph=(99 if KSKIP0 else 0))
    ada_b = din("ada_b", [2, 6 * D])
    n1g = din("n1g", [2, D])
    n2g = din("n2g", [2, D])
    wqkv = din("wqkv", [D, 3 * D], minph=1)
    wo = din("wo", [D, D], minph=3)
    qng = din("qng", [1, HD])
    kng = din("kng", [1, HD])
    lamp = din("lamp", [1, 4 * HD])
    subg = din("subg", [1, 256])
    poolw = din("poolw", [4, 512, 512])
    pools = din("pools", [1, D])
    wgu = din("wgu", [D, 2 * DFF], minph=4)
    wdn = din("wdn", [DFF, D], minph=4)
    wrT = din("wrT", [NE, D])
    br = din("br", [1, NE])
    mgu = din("mgu", [NE, D, 2 * DFE], minph=7)
    mdn = din("mdn", [NE, DFE, D], minph=7)
    ident = din("ident", [128, 128])
    apool = din("apool", [20, 128, 3 * 4 * 128])

    yp = dout("yp", [512, D])
    ys = dout("ys", [2048, D])
    sk = dout("sk", [512, D])
    sv = dout("sv", [512, D])

    mods = din("mods_in", [2, 2, 6 * D]) if KSKIP0 else dscr("mods", [2, 2, 6 * D], F32)
    kT = dscr("kT", [16, 128, NKEY], BF16)
    kTp = dscr("kTp", [16, 128, 512], BF16)
    qT = dscr("qT", [16, 128, NSQ * 128], BF16)
    qTp = dscr("qTp", [16, 128, 512], BF16)
    vall = dscr("vall", [NKEY, D], BF16)
    vp = dscr("vp", [512, D], BF16)
    oall = dscr("oall", [21 * 128, D], BF16)
    x1h = dscr("x1h", [128, D], F32)
    h1all = dscr("h1all", [21 * 128, D], BF16)
    combd = dscr("combd", [20 * 128, NE], F32)

    stack = ExitStack()
    with stack:
        S = Sched(nc, stack)
        A = Arena(nc)
        psf = []
        for i in range(8):
            p = nc.alloc_psum_tensor("ps%d" % i, [128, 512], F32)
            psf.append(T(p[:], "ps%d" % i, excl=True))

        def ps16(i):
            return psf[i].ap.bitcast(BF16)

        def V(eng, method, reads, writes, **kw):
            S.op(eng, (lambda e, m=method, kw=kw: getattr(e, m)(**kw)), reads=reads, writes=writes)

        def G(eng, insts, reads, writes):
            def body(e, insts=insts):
                ins = None
                for m, kw in insts:
                    ins = getattr(e, m)(**kw)
                return ins
            S.op(eng, body, reads=reads, writes=writes)

        def dma(eng, out_ap, in_ap, reads, writes, key, **kw):
            S.op(eng, (lambda e, o=out_ap, i=in_ap, kw=kw: e.dma_start(out=o, in_=i, **kw)),
                 reads=reads, writes=writes, dma=key)

        def rstd_from_ss(ss, rs, scale):
            V("dve", "tensor_scalar", [ss], [rs], out=rs.ap, in0=ss.ap, scalar1=scale, scalar2=EPS, op0=ALU.mult, op1=ALU.add)
            V("act", "sqrt", [rs], [rs], out=rs.ap, in_=rs.ap)
            V("dve", "reciprocal", [rs], [rs], out=rs.ap, in_=rs.ap)

        def norm_mod(xt, junk, ss, rs, Ab, shb, out_t):
            V("act", "activation", [xt], [junk, ss], out=junk.ap, in_=xt.ap, func=AF.Square, accum_out=ss.ap)
            rstd_from_ss(ss, rs, 1.0 / D)
            V("dve", "scalar_tensor_tensor", [xt, rs, Ab], [xt], out=xt.ap, in0=xt.ap, scalar=rs.ap, in1=Ab.ap, op0=ALU.mult, op1=ALU.mult)
            V("dve", "tensor_tensor", [xt, shb], [out_t], out=out_t.ap, in0=xt.ap, in1=shb.ap, op=ALU.add)

        def transpose16(src, dstT, col0, pbanks, dst_toks):
            for half in range(2):
                pb = pbanks[half]
                insts = []
                for j in range(8):
                    kc = half * 8 + j
                    insts.append(("transpose", dict(out=ps16(pb)[:, j * 128:(j + 1) * 128], in_=src.ap[:, kc * 128:(kc + 1) * 128], identity=idb.ap)))
                G("pe", insts, [src, idb], [psf[pb]])
                o = dstT.ap[:, half * 8:(half + 1) * 8, col0:col0 + 128]
                i = ps16(pb).rearrange("p (k t) -> p k t", k=8)
                if half == 0:
                    V("dve", "tensor_copy", [psf[pb]], [dst_toks[0]], out=o, in_=i)
                else:
                    V("act", "copy", [psf[pb]], [dst_toks[1]], out=o, in_=i)

        def load_bcast(dst, src_row_ap, key):
            dma("sp", dst.ap, src_row_ap.partition_broadcast(128), [], [dst], key)

        def make_Ab(dst, tmp, sc_row, g_row, key):
            load_bcast(dst, sc_row, key)
            load_bcast(tmp, g_row, key + "g")
            V("dve", "scalar_tensor_tensor", [dst, tmp], [dst], out=dst.ap, in0=dst.ap, scalar=1.0, in1=tmp.ap, op0=ALU.add, op1=ALU.mult)

        def modrow(L, c, idx):
            return mods[L, c:c + 1, idx * D:(idx + 1) * D]

        def resid(i):
            if i < 4:
                return yp[i * 128:(i + 1) * 128, :]
            if i < 20:
                return ys[(i - 4) * 128:(i - 3) * 128, :]
            return x1h

        def xin(i):
            if i < 4:
                return xp[i * 128:(i + 1) * 128, :]
            return xs[(i - 4) * 128:(i - 3) * 128, :]

        def wview(ap2d):
            return ap2d.rearrange("(k p) n -> p k n", p=128)

        idf = A.alloc("idf", [128, 128], F32)
        idb = A.alloc("idb", [128, 128], BF16)
        dma("sp", idf.ap, ident, [], [idf], "idf")
        V("dve", "tensor_copy", [idf], [idb], out=idb.ap, in_=idf.ap)
        base0 = A.off

        def phase0():
            A.reset(base0)
            cT = A.alloc("cT", [128, 32], F32)
            cb = [A.alloc("cb%d" % c, [128, 16, 128], BF16) for c in range(2)]
            dma("sp", cT.ap, condT, [], [cT], "cT")
            V("act", "activation", [cT], [cT], out=cT.ap, in_=cT.ap, func=AF.Silu)
            for c in range(2):
                V("dve", "tensor_copy", [cT], [cb[c]], out=cb[c].ap, in_=cT.ap[:, c * 16:(c + 1) * 16].unsqueeze(2).to_broadcast([128, 16, 128]))
            wst = [A.alloc("adaw%d" % i, [128, 16, 512], BF16) for i in range(2)]
            bst = [A.alloc("adab%d" % i, [128, 512], F32) for i in range(2)]
            mst = [A.alloc("adam%d" % i, [128, 512], F32) for i in range(4)]
            it = 0
            for L in range(2):
                for n in range(24):
                    w_t = wst[it % 2]
                    b_t = bst[it % 2]
                    dma("pool", w_t.ap, wview(ada_w[L, :, n * 512:(n + 1) * 512]), [], [w_t], "adaw%d" % (it % 2))
                    load_bcast(b_t, ada_b[L:L + 1, n * 512:(n + 1) * 512], "adab%d" % (it % 2))
                    for c in range(2):
                        pb = (it * 2 + c) % 4
                        insts = [("matmul", dict(out=psf[pb].ap, lhsT=cb[c].ap[:, kc, :], rhs=w_t.ap[:, kc, :], start=(kc == 0), stop=(kc == 15))) for kc in range(16)]
                        G("pe", insts, [cb[c], w_t], [psf[pb]])
                        m_t = mst[pb]
                        V("dve", "tensor_tensor", [psf[pb], b_t], [m_t], out=m_t.ap, in0=psf[pb].ap, in1=b_t.ap, op=ALU.add)
                        dma("sp", mods[L, c:c + 1, n * 512:(n + 1) * 512], m_t.ap[0:1, :], [m_t], [], "adam%d" % pb)
                    it += 1
            S.barrier()

        def phaseA():
            A.reset(base0)
            Ab = [A.alloc("A1b%d" % c, [128, D], F32) for c in range(2)]
            shb = [A.alloc("sh1b%d" % c, [128, D], F32) for c in range(2)]
            gq = A.alloc("gq", [128, HD], F32)
            gk = A.alloc("gk", [128, HD], F32)
            xt_s = [A.alloc("xt%d" % i, [128, D], F32) for i in range(2)]
            junk = A.alloc("junk", [128, D], BF16)
            hb_s = [A.alloc("hb%d" % i, [128, D], BF16) for i in range(2)]
            ss_s = [A.alloc("ss%d" % i, [128, 1], F32) for i in range(2)]
            rs_s = [A.alloc("rs%d" % i, [128, 1], F32) for i in range(2)]
            TB = 9
            hT = A.alloc("hT", [128, 16, TB * 128], BF16)
            hTtok = [[T(None, "hT_%d_%d" % (j, q)) for q in range(2)] for j in range(TB)]
            wq_s = [A.alloc("wq%d" % i, [128, 16, 512], BF16) for i in range(2)]
            rc_t = A.alloc("rc", [128, TB, 128], F32)
            rsn_t = A.alloc("rsn", [128, TB, 128], F32)
            NSL = 4
            e1 = [A.alloc("e1_%d" % i, [128, 512], F32) for i in range(NSL)]
            e2 = [A.alloc("e2_%d" % i, [128, 512], F32) for i in range(NSL)]
            e3 = [A.alloc("e3_%d" % i, [128, 512], F32) for i in range(NSL)]
            e4 = [A.alloc("e4_%d" % i, [128, 512], BF16) for i in range(NSL)]
            ess = [A.alloc("ess%d" % i, [128, 4], F32) for i in range(NSL)]
            stg = [A.alloc("stg%d" % i, [128, 4, TB * 128], BF16) for i in range(2)]
            vst = [A.alloc("vst%d" % i, [128, 512], BF16) for i in range(2)]
            vsf = [A.alloc("vsf%d" % i, [128, 512], F32) for i in range(2)]
            make_Ab(Ab[0], xt_s[0], modrow(0, 0, 1), n1g[0:1, :], "A1b0")
            make_Ab(Ab[1], xt_s[1], modrow(0, 1, 1), n1g[0:1, :], "A1b1")
            load_bcast(shb[0], modrow(0, 0, 0), "sh1b0")
            load_bcast(shb[1], modrow(0, 1, 0), "sh1b1")
            load_bcast(gq, qng, "gq")
            load_bcast(gk, kng, "gk")

            for t4 in range(0 if "cache" in KASKIP else 4):
                xt = xt_s[t4 % 2]
                hb = hb_s[t4 % 2]
                vb = e4[t4 % 2]
                for q4 in range(4):
                    dma("pool", vb.ap, cv[t4 * 128:(t4 + 1) * 128, q4 * 512:(q4 + 1) * 512], [], [vb], "cvl%d" % (t4 % 2))
                    dma("sp", vall[t4 * 128:(t4 + 1) * 128, q4 * 512:(q4 + 1) * 512], vb.ap, [vb], [], "cvs%d" % (t4 % 2))
                dma("sp", xt.ap, ck[t4 * 128:(t4 + 1) * 128, :], [], [xt], "xt%d" % (t4 % 2))
                V("dve", "tensor_copy", [xt], [hb], out=hb.ap, in_=xt.ap)
                transpose16(hb, hT, 0, (0, 1), hTtok[0])
                dma("sp", kT[:, :, t4 * 128:(t4 + 1) * 128].rearrange("g d t -> d g t"), hT.ap[:, :, 0:128], hTtok[0], [], "ckst")

            tilesA = []
            for i in range(4):
                tilesA.append(dict(kind="p", idx=i, x=xp[i * 128:(i + 1) * 128, :], c=0, q=True))
            for i in range(NSA):
                tilesA.append(dict(kind="s", idx=i, x=xs[i * 128:(i + 1) * 128, :], c=1, q=(i < NSQ)))
            nblk = min(len(tilesA) // TB, KABLK)
            wit = 0
            eit = 0
            tit = 0
            pending = []
            for blk in range(nblk):
                tl = tilesA[blk * TB:(blk + 1) * TB]
                js = [j for j, t in enumerate(tl) if t["kind"] == "s"]
                if js:
                    j0 = js[0]
                    r0 = tl[j0]["idx"] * 128
                    ns = len(js)
                    dma("sp", rc_t.ap[:, j0:j0 + ns, :], ropeC[r0:r0 + ns * 128, :].rearrange("(j p) f -> p j f", p=128), [], [rc_t], "rc")
                    dma("sp", rsn_t.ap[:, j0:j0 + ns, :], ropeS[r0:r0 + ns * 128, :].rearrange("(j p) f -> p j f", p=128), [], [rsn_t], "rsn")
                for j, t in enumerate(tl):
                    xt = xt_s[j % 2]
                    hb = hb_s[j % 2]
                    dma("sp", xt.ap, t["x"], [], [xt], "xt%d" % (j % 2))
                    norm_mod(xt, junk, ss_s[j % 2], rs_s[j % 2], Ab[t["c"]], shb[t["c"]], hb)
                    transpose16(hb, hT, j * 128, (0, 1), hTtok[j])
                anyq = any(t["q"] for t in tl)
                for ch in range(12):
                    if ch < 4 and not anyq:
                        continue
                    w_t = wq_s[wit % 2]
                    dma("pool", w_t.ap, wview(wqkv[:, ch * 512:(ch + 1) * 512]), [], [w_t], "wq%d" % (wit % 2))
                    wit += 1
                    kind = "q" if ch < 4 else ("k" if ch < 8 else "v")
                    if kind in KASKIP:
                        continue
                    cc = ch % 4
                    st = stg[ch % 2]
                    used = []
                    for j, t in enumerate(tl):
                        if kind == "q" and not t["q"]:
                            continue
                        used.append(j)
                        pb = 2 + (eit % 3)
                        sl = eit % (2 if kind == "v" else NSL)
                        eit += 1
                        insts = [("matmul", dict(out=psf[pb].ap, lhsT=hT.ap[:, kc, j * 128:(j + 1) * 128], rhs=w_t.ap[:, kc, :], start=(kc == 0), stop=(kc == 15))) for kc in range(16)]
                        G("pe", insts, [hTtok[j][0], hTtok[j][1], w_t], [psf[pb]])
                        while len(pending) > 2:
                            pending.pop(0)()
                        P = psf[pb]
                        if kind == "v":
                            if t["kind"] == "p":
                                r0 = t["idx"] * 128
                                V("dve", "tensor_copy", [P], [vsf[sl]], out=vsf[sl].ap, in_=P.ap)
                                dma("sp", sv[r0:r0 + 128, cc * 512:(cc + 1) * 512], vsf[sl].ap, [vsf[sl]], [], "vsf%d" % sl)
                                V("act", "copy", [P], [vst[sl]], out=vst[sl].ap, in_=P.ap)
                                dma("sp", vp[r0:r0 + 128, cc * 512:(cc + 1) * 512], vst[sl].ap, [vst[sl]], [], "vst%d" % sl)
                            else:
                                V("act", "copy", [P], [vst[sl]], out=vst[sl].ap, in_=P.ap)
                                r0 = PAST + t["idx"] * 128
                                dma("sp", vall[r0:r0 + 128, cc * 512:(cc + 1) * 512], vst[sl].ap, [vst[sl]], [], "vst%d" % sl)
                            continue
                        g_t = gq if kind == "q" else gk
                        g4 = lambda a: a.rearrange("p (g d) -> p g d", g=4)
                        V("act", "copy", [P], [e2[sl]], out=e2[sl].ap, in_=P.ap)
                        V("act", "activation", [e2[sl]], [e1[sl]], out=e1[sl].ap, in_=e2[sl].ap, func=AF.Square)
                        V("dve", "tensor_reduce", [e1[sl]], [ess[sl]], out=ess[sl].ap, in_=g4(e1[sl].ap), axis=AX.X, op=ALU.add)
                        rstd_from_ss(ess[sl], ess[sl], 1.0 / HD)
                        V("dve", "tensor_tensor", [e2[sl], ess[sl]], [e2[sl]], out=g4(e2[sl].ap), in0=g4(e2[sl].ap), in1=ess[sl].ap.unsqueeze(2).to_broadcast([128, 4, HD]), op=ALU.mult)
                        V("dve", "tensor_tensor", [e2[sl], g_t], [e2[sl]], out=g4(e2[sl].ap), in0=g4(e2[sl].ap), in1=g_t.ap.unsqueeze(1).to_broadcast([128, 4, HD]), op=ALU.mult)
                        if t["kind"] == "p" or "rope" in KASKIP:
                            if kind == "k" and t["kind"] == "p":
                                r0 = t["idx"] * 128
                                dma("sp", sk[r0:r0 + 128, cc * 512:(cc + 1) * 512], e2[sl].ap, [e2[sl]], [], "e2_%d" % sl)
                            V("dve", "tensor_copy", [e2[sl]], [e4[sl]], out=e4[sl].ap, in_=e2[sl].ap)
                        else:
                            Cb = rc_t.ap[:, j, :]
                            Sb = rsn_t.ap[:, j, :].rearrange("p (a j f) -> p a j f", a=2, j=2)
                            x5 = e2[sl].ap.rearrange("p (g a j f) -> p g a j f", g=4, a=2, j=2)
                            t5 = e3[sl].ap.rearrange("p (g a j f) -> p g a j f", g=4, a=2, j=2)
                            V("dve", "tensor_tensor", [e2[sl], rc_t], [e1[sl]], out=g4(e1[sl].ap), in0=g4(e2[sl].ap), in1=Cb.unsqueeze(1).to_broadcast([128, 4, HD]), op=ALU.mult)
                            V("dve", "tensor_tensor", [e2[sl], rsn_t], [e3[sl]], out=t5[:, :, :, 0, :], in0=x5[:, :, :, 1, :], in1=Sb[:, :, 0, :].unsqueeze(1).to_broadcast([128, 4, 2, 32]), op=ALU.mult)
                            V("dve", "tensor_tensor", [e2[sl], rsn_t], [e3[sl]], out=t5[:, :, :, 1, :], in0=x5[:, :, :, 0, :], in1=Sb[:, :, 1, :].unsqueeze(1).to_broadcast([128, 4, 2, 32]), op=ALU.mult)
                            V("dve", "tensor_tensor", [e1[sl], e3[sl]], [e4[sl]], out=e4[sl].ap, in0=e1[sl].ap, in1=e3[sl].ap, op=ALU.add)
                        tpb = 5 + (tit % 3)
                        tit += 1

                        def do_tr(sl=sl, tpb=tpb, st=st, j=j):
                            insts = [("transpose", dict(out=ps16(tpb)[:, g * 128:(g + 1) * 128], in_=e4[sl].ap[:, g * 128:(g + 1) * 128], identity=idb.ap)) for g in range(4)]
                            G("pe", insts, [e4[sl], idb], [psf[tpb]])
                            V("act", "copy", [psf[tpb]], [st], out=st.ap[:, :, j * 128:(j + 1) * 128], in_=ps16(tpb)[:, 0:512].rearrange("p (g t) -> p g t", g=4))
                        pending.append(do_tr)
                    while pending:
                        pending.pop(0)()
                    if kind == "v" or not used:
                        continue
                    j_p = [j for j in used if tl[j]["kind"] == "p"]
                    j_s = [j for j in used if tl[j]["kind"] == "s"]
                    gsl = slice(cc * 4, cc * 4 + 4)
                    if j_p:
                        dst = (qTp if kind == "q" else kTp)
                        c0 = tl[j_p[0]]["idx"] * 128
                        n = len(j_p) * 128
                        dma("sp", dst[gsl, :, c0:c0 + n].rearrange("g d t -> d g t"), st.ap[:, :, j_p[0] * 128:j_p[0] * 128 + n], [st], [], "stg%dp" % (ch % 2))
                    if j_s:
                        i0 = tl[j_s[0]]["idx"]
                        n = len(j_s) * 128
                        if kind == "q":
                            dstap = qT[gsl, :, i0 * 128:i0 * 128 + n]
                        else:
                            dstap = kT[gsl, :, PAST + i0 * 128:PAST + i0 * 128 + n]
                        dma("sp", dstap.rearrange("g d t -> d g t"), st.ap[:, :, j_s[0] * 128:j_s[0] * 128 + n], [st], [], "stg%ds" % (ch % 2))
            S.barrier()

        def phaseB():
            A.reset(base0)
            KT = [A.alloc("KT%d" % i, [128, 2, NKEY], BF16) for i in range(2)]
            VA = [A.alloc("VA%d" % i, [128, 36, 257], BF16) for i in range(2)]
            QT = [A.alloc("QT%d" % i, [128, 2, NSQ * 128], BF16) for i in range(2)]
            KTP = [A.alloc("KTP%d" % i, [128, 2, 512], BF16) for i in range(2)]
            VAP = [A.alloc("VAP%d" % i, [128, 4, 257], BF16) for i in range(2)]
            QTP = [A.alloc("QTP%d" % i, [128, 2, 512], BF16) for i in range(2)]
            PT = [A.alloc("PT%d" % i, [128, 512], BF16) for i in range(4)]
            o0 = [A.alloc("o0_%d" % i, [128, 256], F32) for i in range(4)]
            of = [A.alloc("of%d" % i, [128, 256], F32) for i in range(2)]
            ob = [A.alloc("ob%d" % i, [128, 256], BF16) for i in range(2)]
            osq = [A.alloc("osq%d" % i, [128, 256], F32) for i in range(2)]
            r0t = [A.alloc("r0t%d" % i, [128, 1], F32) for i in range(4)]
            r1t = [A.alloc("r1t%d" % i, [128, 1], F32) for i in range(2)]
            oss = [A.alloc("oss%d" % i, [128, 1], F32) for i in range(2)]
            lpt = A.alloc("lpt", [128, 512], F32)
            lj = A.alloc("lj", [128, 128], F32)
            lsum = A.alloc("lsum", [128, 2], F32)
            nlam = A.alloc("nlam", [128, 1], F32)
            sgb = A.alloc("sgb", [128, 256], F32)
            load_bcast(lpt, lamp, "lpt")
            load_bcast(sgb, subg, "sgb")
            V("dve", "tensor_scalar_mul", [sgb], [sgb], out=sgb.ap, in0=sgb.ap, scalar1=(1.0 - LAMBDA_INIT))
            for q in range(2):
                V("dve", "tensor_tensor", [lpt], [lj], out=lj.ap, in0=lpt.ap[:, (2 * q) * 128:(2 * q + 1) * 128], in1=lpt.ap[:, (2 * q + 1) * 128:(2 * q + 2) * 128], op=ALU.mult)
                V("dve", "tensor_reduce", [lj], [lsum], out=lsum.ap[:, q:q + 1], in_=lj.ap, axis=AX.X, op=ALU.add)
            V("act", "activation", [lsum], [lsum], out=lsum.ap, in_=lsum.ap, func=AF.Exp)
            V("dve", "tensor_tensor", [lsum], [nlam], out=nlam.ap, in0=lsum.ap[:, 1:2], in1=lsum.ap[:, 0:1], op=ALU.subtract)
            V("dve", "tensor_scalar_add", [nlam], [nlam], out=nlam.ap, in0=nlam.ap, scalar1=-LAMBDA_INIT)
            for i in range(2):
                V("dve", "memset", [], [VA[i]], ap=VA[i].ap[:, :, 256:257], constant=1.0)
                V("dve", "memset", [], [VAP[i]], ap=VAP[i].ap[:, :, 256:257], constant=1.0)
            SCALE = HD ** -0.5
            uctr = [0]
            octr = [0]

            def attention(Kt, Vt, Qt, kcol0, nkc, vch0, qcol0, ntile, orow0, h):
                units = [(c, kc) for c in range(2) for kc in range(nkc)]
                n = len(units)
                pend = []
                nq = ntile * 128
                LA = 3
                for idx in range(n + LA):
                    if idx < n:
                        c, kc = units[idx]
                        u = uctr[0]
                        uctr[0] += 1
                        sb_ = 4 + (u % 4)
                        pt = PT[u % 4]
                        V("pe", "matmul", [Kt, Qt], [psf[sb_]], out=psf[sb_].ap[:, 0:nq], lhsT=Kt.ap[:, c, kcol0 + kc * 128:kcol0 + (kc + 1) * 128], rhs=Qt.ap[:, c, qcol0:qcol0 + nq], start=True, stop=True)
                        V("act", "activation", [psf[sb_]], [pt], out=pt.ap[:, 0:nq], in_=psf[sb_].ap[:, 0:nq], func=AF.Exp, scale=SCALE)
                        pend.append((c, kc, pt))
                    if idx >= LA:
                        c, kc, pt = pend[idx - LA]
                        for i in range(ntile):
                            V("pe", "matmul", [pt, Vt], [psf[i]], out=psf[i].ap[:, 0:257], lhsT=pt.ap[:, i * 128:(i + 1) * 128], rhs=Vt.ap[:, vch0 + kc, :], start=(kc == 0), stop=(kc == nkc - 1))
                        if kc != nkc - 1:
                            continue
                        for i in range(ntile):
                            acc = psf[i]
                            if c == 0:
                                V("dve", "reciprocal", [acc], [r0t[i]], out=r0t[i].ap, in_=acc.ap[:, 256:257])
                                V("dve", "tensor_scalar_mul", [acc, r0t[i]], [o0[i]], out=o0[i].ap, in0=acc.ap[:, 0:256], scalar1=r0t[i].ap)
                            else:
                                k = octr[0] % 2
                                octr[0] += 1
                                V("dve", "reciprocal", [acc], [r1t[k]], out=r1t[k].ap, in_=acc.ap[:, 256:257])
                                V("dve", "tensor_tensor", [r1t[k], nlam], [r1t[k]], out=r1t[k].ap, in0=r1t[k].ap, in1=nlam.ap, op=ALU.mult)
                                V("dve", "scalar_tensor_tensor", [acc, r1t[k], o0[i]], [of[k]], out=of[k].ap, in0=acc.ap[:, 0:256], scalar=r1t[k].ap, in1=o0[i].ap, op0=ALU.mult, op1=ALU.add)
                                V("dve", "tensor_tensor", [of[k]], [osq[k]], out=osq[k].ap, in0=of[k].ap, in1=of[k].ap, op=ALU.mult)
                                V("dve", "tensor_reduce", [osq[k]], [oss[k]], out=oss[k].ap, in_=osq[k].ap, axis=AX.X, op=ALU.add)
                                V("dve", "tensor_scalar", [oss[k]], [oss[k]], out=oss[k].ap, in0=oss[k].ap, scalar1=1.0 / 256, scalar2=EPS, op0=ALU.mult, op1=ALU.add)
                                V("act", "activation", [oss[k]], [oss[k]], out=oss[k].ap, in_=oss[k].ap, func=AF.Ln)
                                V("act", "activation", [oss[k]], [oss[k]], out=oss[k].ap, in_=oss[k].ap, func=AF.Exp, scale=-0.5)
                                V("dve", "scalar_tensor_tensor", [of[k], oss[k], sgb], [ob[k]], out=ob[k].ap, in0=of[k].ap, scalar=oss[k].ap, in1=sgb.ap, op0=ALU.mult, op1=ALU.mult)
                                r = orow0 + i * 128
                                dma("sp", oall[r:r + 128, h * 256:(h + 1) * 256], ob[k].ap, [ob[k]], [], "ob%d" % k)

            for h in range(NH):
                sl = h % 2
                dma("sp", KT[sl].ap, kT[2 * h:2 * h + 2, :, :].rearrange("c d t -> d c t"), [], [KT[sl]], "KT%d" % sl)
                dma("sp", QT[sl].ap, qT[2 * h:2 * h + 2, :, :].rearrange("c d t -> d c t"), [], [QT[sl]], "QT%d" % sl)
                dma("sp", VA[sl].ap[:, :, 0:256], vall[:, h * 256:(h + 1) * 256].rearrange("(k p) e -> p k e", p=128), [], [VA[sl]], "VA%d" % sl)
                dma("sp", KTP[sl].ap, kTp[2 * h:2 * h + 2, :, :].rearrange("c d t -> d c t"), [], [KTP[sl]], "KTP%d" % sl)
                dma("sp", QTP[sl].ap, qTp[2 * h:2 * h + 2, :, :].rearrange("c d t -> d c t"), [], [QTP[sl]], "QTP%d" % sl)
                dma("sp", VAP[sl].ap[:, :, 0:256], vp[:, h * 256:(h + 1) * 256].rearrange("(k p) e -> p k e", p=128), [], [VAP[sl]], "VAP%d" % sl)
                for sq in range(2):
                    attention(KTP[sl], VAP[sl], QTP[sl], sq * 256, 2, sq * 2, sq * 256, 2, sq * 256, h)
                for qb in range(0, NSQ, 4):
                    nt = min(4, NSQ - qb)
                    attention(KT[sl], VA[sl], QT[sl], 0, 36, 0, qb * 128, nt, 512 + qb * 128, h)
            S.barrier()

        def phaseC():
            A.reset(base0)
            wob = A.alloc("wob", [128, 16, D], BF16)
            g1b = [A.alloc("g1b%d" % c, [128, D], F32) for c in range(2)]
            ot_s = [A.alloc("ot%d" % i, [128, D], BF16) for i in range(2)]
            oT_s = [A.alloc("oT%d" % i, [128, 16, 128], BF16) for i in range(2)]
            xt_s = [A.alloc("xt%d" % i, [128, D], F32) for i in range(2)]
            tm_s = [A.alloc("tm%d" % i, [128, 512], F32) for i in range(2)]
            wobq = [T(None, "wobq%d" % q) for q in range(4)]
            for q in range(4):
                dma("pool", wob.ap[:, :, q * 512:(q + 1) * 512], wview(wo[:, q * 512:(q + 1) * 512]), [], [wobq[q]], "wob%d" % q)
            for c in range(2):
                load_bcast(g1b[c], modrow(0, c, 2), "g1b%d" % c)
            ectr = 0
            for i in range(21):
                sl = i % 2
                c = 0 if i < 4 else 1
                xt = xt_s[sl]
                dma("sp", ot_s[sl].ap, oall[i * 128:(i + 1) * 128, :], [], [ot_s[sl]], "ot%d" % sl)
                dma("sp", xt.ap, xin(i), [], [xt], "xt%d" % sl)
                transpose16(ot_s[sl], oT_s[sl], 0, (0, 1), [oT_s[sl], oT_s[sl]])
                for q in range(4):
                    pb = 2 + (ectr % 3)
                    tm = tm_s[ectr % 2]
                    ectr += 1
                    insts = [("matmul", dict(out=psf[pb].ap, lhsT=oT_s[sl].ap[:, kc, :], rhs=wob.ap[:, kc, q * 512:(q + 1) * 512], start=(kc == 0), stop=(kc == 15))) for kc in range(16)]
                    G("pe", insts, [oT_s[sl], wobq[q]], [psf[pb]])
                    V("dve", "tensor_tensor", [psf[pb], g1b[c]], [tm], out=tm.ap, in0=psf[pb].ap, in1=g1b[c].ap[:, q * 512:(q + 1) * 512], op=ALU.mult)
                    V("dve", "tensor_tensor", [tm, xt], [xt], out=xt.ap[:, q * 512:(q + 1) * 512], in0=xt.ap[:, q * 512:(q + 1) * 512], in1=tm.ap, op=ALU.add)
                dma("sp", resid(i), xt.ap, [xt], [], "xo%d" % sl)
            S.barrier()

        def ffn_phase(L, blocks, experts, use_comb, MT=8):
            A.reset(base0)
            Ab2 = A.alloc("A2b", [128, D], F32)
            sh2 = A.alloc("sh2b", [128, D], F32)
            xt_s = [A.alloc("xt%d" % i, [128, D], F32) for i in range(2)]
            junk = A.alloc("junk", [128, D], BF16)
            hb_s = [A.alloc("hb%d" % i, [128, D], BF16) for i in range(2)]
            ss_s = [A.alloc("ss%d" % i, [128, 1], F32) for i in range(2)]
            rs_s = [A.alloc("rs%d" % i, [128, 1], F32) for i in range(2)]
            h2T = A.alloc("h2T", [128, 16, MT * 128], BF16)
            h2tok = [[T(None, "h2T_%d_%d" % (j, q)) for q in range(2)] for j in range(MT)]
            hidT = A.alloc("hidT", [128, 22, MT * 128], BF16)
            wg_s = [A.alloc("wg%d" % i, [128, 16, 256], BF16) for i in range(2)]
            wu_s = [A.alloc("wu%d" % i, [128, 16, 256], BF16) for i in range(2)]
            wd_s = [A.alloc("wd%d" % i, [128, 22, 256], BF16) for i in range(2)]
            sil = [A.alloc("sil%d" % i, [128, 512], F32) for i in range(2)]
            ct_s = [A.alloc("ct%d" % i, [128, 256], F32) for i in range(4)]
            cmb = A.alloc("cmb", [128, MT, NE], F32)
            ytok = {}

            gu_jobs = []
            d_jobs = []
            for bi in range(len(blocks)):
                for ex in experts:
                    for cb_ in range(11):
                        gu_jobs.append((ex, cb_))
                    for q8 in range(8):
                        d_jobs.append((ex, q8))
            gu_issued = [0]
            d_issued = [0]

            def issue_gu(upto):
                while gu_issued[0] <= upto and gu_issued[0] < len(gu_jobs):
                    k = gu_issued[0]
                    ex, cb_ = gu_jobs[k]
                    dma("pool", wg_s[k % 2].ap, wview(ex["g"](cb_ * 256, 256)), [], [wg_s[k % 2]], "wg%d" % (k % 2))
                    dma("pool", wu_s[k % 2].ap, wview(ex["u"](cb_ * 256, 256)), [], [wu_s[k % 2]], "wu%d" % (k % 2))
                    gu_issued[0] += 1

            def issue_d(upto):
                while d_issued[0] <= upto and d_issued[0] < len(d_jobs):
                    k = d_issued[0]
                    ex, q8 = d_jobs[k]
                    dma("pool", wd_s[k % 2].ap, wview(ex["d"](q8 * 256, 256)), [], [wd_s[k % 2]], "wd%d" % (k % 2))
                    d_issued[0] += 1

            gk_ = 0
            dk_ = 0
            sctr = 0
            gctr = 0
            for (c, tiles) in blocks:
                nt = len(tiles)
                ntok = nt * 128
                subs = []
                o_ = 0
                while o_ < ntok:
                    n_ = min(512, ntok - o_)
                    subs.append((o_, n_))
                    o_ += n_
                ns_ = len(subs)
                make_Ab(Ab2, xt_s[1], modrow(L, c, 4), n2g[L:L + 1, :], "A2b")
                load_bcast(sh2, modrow(L, c, 3), "sh2b")
                if use_comb:
                    r0 = tiles[0] * 128
                    dma("sp", cmb.ap[:, 0:nt, :], combd[r0:r0 + ntok, :].rearrange("(j p) e -> p j e", p=128), [], [cmb], "cmb")
                for j, ti in enumerate(tiles):
                    xt = xt_s[j % 2]
                    dma("sp", xt.ap, resid(ti), [], [xt], "xt%d" % (j % 2))
                    norm_mod(xt, junk, ss_s[j % 2], rs_s[j % 2], Ab2, sh2, hb_s[j % 2])
                    transpose16(hb_s[j % 2], h2T, j * 128, (6, 7), h2tok[j])
                load_bcast(Ab2, modrow(L, c, 5), "g2b")
                h2reads = [b for j in range(nt) for b in h2tok[j]]
                for ei, ex in enumerate(experts):
                    issue_d(dk_)
                    for cb_ in range(11):
                        issue_gu(gk_ + 1)
                        wg = wg_s[gk_ % 2]
                        wu = wu_s[gk_ % 2]
                        gk_ += 1
                        for half in range(2):
                            jf = cb_ * 2 + half
                            for (wt, base) in ((wg, 0), (wu, 3)):
                                insts = []
                                for kc in range(16):
                                    for si, (o_, n_) in enumerate(subs):
                                        insts.append(("matmul", dict(out=psf[base + si].ap[:, 0:n_], lhsT=wt.ap[:, kc, half * 128:(half + 1) * 128], rhs=h2T.ap[:, kc, o_:o_ + n_], start=(kc == 0), stop=(kc == 15))))
                                G("pe", insts, h2reads + [wt], [psf[base + si] for si in range(ns_)])
                            for si, (o_, n_) in enumerate(subs):
                                st = sil[sctr % 2]
                                sctr += 1
                                V("act", "activation", [psf[si]], [st], out=st.ap[:, 0:n_], in_=psf[si].ap[:, 0:n_], func=AF.Silu)
                                V("dve", "tensor_tensor", [st, psf[3 + si]], [hidT], out=hidT.ap[:, jf, o_:o_ + n_], in0=st.ap[:, 0:n_], in1=psf[3 + si].ap[:, 0:n_], op=ALU.mult)
                    for q8 in range(8):
                        issue_d(dk_ + 1)
                        wd = wd_s[dk_ % 2]
                        dk_ += 1
                        for j, ti in enumerate(tiles):
                            pb = 6 + (gctr % 2)
                            ct = ct_s[gctr % 4]
                            ckey = "ct%d" % (gctr % 4)
                            gctr += 1
                            insts = [("matmul", dict(out=psf[pb].ap[:, 0:256], lhsT=hidT.ap[:, f, j * 128:(j + 1) * 128], rhs=wd.ap[:, f, :], start=(f == 0), stop=(f == 21))) for f in range(22)]
                            G("pe", insts, [hidT, wd], [psf[pb]])
                            if use_comb:
                                V("dve", "scalar_tensor_tensor", [psf[pb], cmb, Ab2], [ct], out=ct.ap, in0=psf[pb].ap[:, 0:256], scalar=cmb.ap[:, j, ei:ei + 1], in1=Ab2.ap[:, q8 * 256:(q8 + 1) * 256], op0=ALU.mult, op1=ALU.mult)
                            else:
                                V("dve", "tensor_tensor", [psf[pb], Ab2], [ct], out=ct.ap, in0=psf[pb].ap[:, 0:256], in1=Ab2.ap[:, q8 * 256:(q8 + 1) * 256], op=ALU.mult)
                            yk = ytok.setdefault((ti, q8), T(None, "y_%d_%d" % (ti, q8)))
                            dma("pool", resid(ti)[:, q8 * 256:(q8 + 1) * 256], ct.ap, [ct], [yk], ckey, accum_op=ALU.add)
            S.barrier()

        def dense_expert(half):
            return dict(
                g=lambda c0, n, half=half: wgu[:, half * DFE + c0:half * DFE + c0 + n],
                u=lambda c0, n, half=half: wgu[:, DFF + half * DFE + c0:DFF + half * DFE + c0 + n],
                d=lambda c0, n, half=half: wdn[half * DFE:(half + 1) * DFE, c0:c0 + n],
            )

        def moe_expert(ei):
            return dict(
                g=lambda c0, n, ei=ei: mgu[ei, :, c0:c0 + n],
                u=lambda c0, n, ei=ei: mgu[ei, :, DFE + c0:DFE + c0 + n],
                d=lambda c0, n, ei=ei: mdn[ei, :, c0:c0 + n],
            )

        def phaseE1():
            A.reset(base0)
            Ab = A.alloc("A1b", [128, D], F32)
            shb1 = A.alloc("sh1b", [128, D], F32)
            xt_s = [A.alloc("xt%d" % i, [128, D], F32) for i in range(2)]
            junk = A.alloc("junk", [128, D], BF16)
            hb_s = [A.alloc("hb%d" % i, [128, D], BF16) for i in range(2)]
            ss_s = [A.alloc("ss%d" % i, [128, 1], F32) for i in range(2)]
            rs_s = [A.alloc("rs%d" % i, [128, 1], F32) for i in range(2)]
            for c, tiles in ((0, [0, 1, 2, 3]), (1, list(range(4, 21)))):
                make_Ab(Ab, xt_s[1], modrow(1, c, 1), n1g[1:2, :], "A1b")
                load_bcast(shb1, modrow(1, c, 0), "sh1b")
                for j, ti in enumerate(tiles):
                    xt = xt_s[j % 2]
                    dma("sp", xt.ap, resid(ti), [], [xt], "xt%d" % (j % 2))
                    norm_mod(xt, junk, ss_s[j % 2], rs_s[j % 2], Ab, shb1, hb_s[j % 2])
                    dma("sp", h1all[ti * 128:(ti + 1) * 128, :], hb_s[j % 2].ap, [hb_s[j % 2]], [], "h1o%d" % (j % 2))
            S.barrier()

        def phaseE2():
            A.reset(base0)
            pwb = A.alloc("pwb", [128, 4, 4, 512], BF16)
            wrb = A.alloc("wrb", [128, NE, D], F32)
            brb = A.alloc("brb", [128, NE], F32)
            psg = A.alloc("psg", [128, D], F32)
            Ab2 = A.alloc("A2b", [128, D], F32)
            sh2 = A.alloc("sh2b", [128, D], F32)
            h3_s = [A.alloc("h3_%d" % i, [128, 3, D], BF16) for i in range(2)]
            ap_s = [A.alloc("ap%d" % i, [128, 3, 4, 128], BF16) for i in range(2)]
            xt_s = [A.alloc("xt%d" % i, [128, D], F32) for i in range(2)]
            junk = A.alloc("junk", [128, D], BF16)
            rtmp = A.alloc("rtmp", [128, D], F32)
            rtm2 = [rtmp, A.alloc("rtmp2", [128, D], F32)]
            pT = A.alloc("pT", [128, 16, 128], BF16)
            tm_s = [A.alloc("tm%d" % i, [128, 512], F32) for i in range(2)]
            ss_s = [A.alloc("ss%d" % i, [128, 1], F32) for i in range(2)]
            rs_s = [A.alloc("rs%d" % i, [128, 1], F32) for i in range(2)]
            lg = [A.alloc("lg%d" % i, [128, NE], F32) for i in range(2)]
            l2 = [A.alloc("l2_%d" % i, [128, NE], F32) for i in range(2)]
            mk1 = [A.alloc("mk1_%d" % i, [128, NE], F32) for i in range(2)]
            mk2 = [A.alloc("mk2_%d" % i, [128, NE], F32) for i in range(2)]
            m1 = [A.alloc("m1_%d" % i, [128, 1], F32) for i in range(2)]
            m2 = [A.alloc("m2_%d" % i, [128, 1], F32) for i in range(2)]
            gt = [A.alloc("gt%d" % i, [128, 2], F32) for i in range(2)]
            cmo = [A.alloc("cmo%d" % i, [128, NE], F32) for i in range(2)]
            for g in range(4):
                dma("pool", pwb.ap[:, g, :, :], wview(poolw[g]), [], [pwb], "pwb")
            dma("sp", wrb.ap.rearrange("p e d -> p (e d)"), wrT.rearrange("(o e) d -> o (e d)", o=1).partition_broadcast(128), [], [wrb], "wrb")
            load_bcast(brb, br, "brb")

            def prev_next(ti):
                if ti < 4:
                    base = (ti // 2) * 2
                    return (base, ti, base + 1)
                i = ti - 4
                p = 20 if i == 0 else ti - 1
                n = 20 if i == 15 else ti + 1
                return (p, ti, n)

            ectr = 0
            for c, tiles in ((0, [0, 1, 2, 3]), (1, list(range(4, 20)))):
                load_bcast(psg, pools, "psg")
                load_bcast(sh2, modrow(1, c, 2), "psg1")
                V("dve", "tensor_tensor", [psg, sh2], [psg], out=psg.ap, in0=psg.ap, in1=sh2.ap, op=ALU.mult)
                make_Ab(Ab2, rtmp, modrow(1, c, 4), n2g[1:2, :], "A2b")
                load_bcast(sh2, modrow(1, c, 3), "sh2b")
                for j, ti in enumerate(tiles):
                    sl = j % 2
                    h3 = h3_s[sl]
                    apt = ap_s[sl]
                    xt = xt_s[sl]
                    pr = prev_next(ti)
                    for s3 in range(3):
                        dma("sp", h3.ap[:, s3, :], h1all[pr[s3] * 128:(pr[s3] + 1) * 128, :], [], [h3], "h3_%d_%d" % (sl, s3))
                    dma("pool", apt.ap, apool[ti].rearrange("p (s g t) -> p s g t", s=3, g=4), [], [apt], "ap%d" % sl)
                    dma("sp", xt.ap, resid(ti), [], [xt], "xt%d" % sl)
                    for g in range(4):
                        insts = []
                        for cc in range(4):
                            for s3 in range(3):
                                insts.append(("matmul", dict(out=psf[g].ap[:, cc * 128:(cc + 1) * 128], lhsT=h3.ap[:, s3, g * 512 + cc * 128:g * 512 + (cc + 1) * 128], rhs=apt.ap[:, s3, g, :], start=(s3 == 0), stop=(s3 == 2))))
                        G("pe", insts, [h3, apt], [psf[g]])
                        if g % 2 == 0:
                            V("dve", "tensor_copy", [psf[g]], [pT], out=pT.ap[:, g * 4:(g + 1) * 4, :], in_=psf[g].ap.rearrange("p (k t) -> p k t", k=4))
                        else:
                            V("act", "copy", [psf[g]], [pT], out=pT.ap[:, g * 4:(g + 1) * 4, :], in_=psf[g].ap.rearrange("p (k t) -> p k t", k=4))
                    for g in range(4):
                        pb = 4 + (ectr % 3)
                        tm = tm_s[ectr % 2]
                        ectr += 1
                        insts = [("matmul", dict(out=psf[pb].ap, lhsT=pT.ap[:, g * 4 + cc, :], rhs=pwb.ap[:, g, cc, :], start=(cc == 0), stop=(cc == 3))) for cc in range(4)]
                        G("pe", insts, [pT, pwb], [psf[pb]])
                        V("dve", "tensor_tensor", [psf[pb], psg], [tm], out=tm.ap, in0=psf[pb].ap, in1=psg.ap[:, g * 512:(g + 1) * 512], op=ALU.mult)
                        V("dve", "tensor_tensor", [tm, xt], [xt], out=xt.ap[:, g * 512:(g + 1) * 512], in0=xt.ap[:, g * 512:(g + 1) * 512], in1=tm.ap, op=ALU.add)
                    dma("sp", resid(ti), xt.ap, [xt], [], "xo%d" % sl)
                    V("act", "activation", [xt], [junk, ss_s[sl]], out=junk.ap, in_=xt.ap, func=AF.Square, accum_out=ss_s[sl].ap)
                    rstd_from_ss(ss_s[sl], rs_s[sl], 1.0 / D)
                    V("dve", "scalar_tensor_tensor", [xt, rs_s[sl], Ab2], [xt], out=xt.ap, in0=xt.ap, scalar=rs_s[sl].ap, in1=Ab2.ap, op0=ALU.mult, op1=ALU.mult)
                    V("dve", "tensor_tensor", [xt, sh2], [xt], out=xt.ap, in0=xt.ap, in1=sh2.ap, op=ALU.add)
                    for ei in range(NE):
                        rt_ = rtm2[ei % 2]
                        V("dve", "tensor_tensor", [xt, wrb], [rt_], out=rt_.ap, in0=xt.ap, in1=wrb.ap[:, ei, :], op=ALU.mult)
                        V("act", "activation", [rt_], [junk, lg[sl]], out=junk.ap, in_=rt_.ap, func=AF.Copy, accum_out=lg[sl].ap[:, ei:ei + 1])
                    V("dve", "tensor_tensor", [lg[sl], brb], [lg[sl]], out=lg[sl].ap, in0=lg[sl].ap, in1=brb.ap, op=ALU.add)
                    V("dve", "tensor_reduce", [lg[sl]], [m1[sl]], out=m1[sl].ap, in_=lg[sl].ap, axis=AX.X, op=ALU.max)
                    V("dve", "tensor_single_scalar", [lg[sl], m1[sl]], [mk1[sl]], out=mk1[sl].ap, in_=lg[sl].ap, scalar=m1[sl].ap, op=ALU.is_equal)
                    V("dve", "scalar_tensor_tensor", [mk1[sl], lg[sl]], [l2[sl]], out=l2[sl].ap, in0=mk1[sl].ap, scalar=-1e30, in1=lg[sl].ap, op0=ALU.mult, op1=ALU.add)
                    V("dve", "tensor_reduce", [l2[sl]], [m2[sl]], out=m2[sl].ap, in_=l2[sl].ap, axis=AX.X, op=ALU.max)
                    V("dve", "tensor_single_scalar", [l2[sl], m2[sl]], [mk2[sl]], out=mk2[sl].ap, in_=l2[sl].ap, scalar=m2[sl].ap, op=ALU.is_equal)
                    V("dve", "tensor_tensor", [m1[sl], m2[sl]], [gt[sl]], out=gt[sl].ap[:, 0:1], in0=m1[sl].ap, in1=m2[sl].ap, op=ALU.subtract)
                    V("dve", "tensor_tensor", [m1[sl], m2[sl], gt[sl]], [gt[sl]], out=gt[sl].ap[:, 1:2], in0=m2[sl].ap, in1=m1[sl].ap, op=ALU.subtract)
                    V("act", "activation", [gt[sl]], [gt[sl]], out=gt[sl].ap, in_=gt[sl].ap, func=AF.Sigmoid)
                    V("dve", "tensor_scalar_mul", [mk1[sl], gt[sl]], [cmo[sl]], out=cmo[sl].ap, in0=mk1[sl].ap, scalar1=gt[sl].ap[:, 0:1])
                    V("dve", "scalar_tensor_tensor", [mk2[sl], gt[sl], cmo[sl]], [cmo[sl]], out=cmo[sl].ap, in0=mk2[sl].ap, scalar=gt[sl].ap[:, 1:2], in1=cmo[sl].ap, op0=ALU.mult, op1=ALU.add)
                    dma("sp", combd[ti * 128:(ti + 1) * 128, :], cmo[sl].ap, [cmo[sl]], [], "cmo%d" % sl)
            S.barrier()

        if not KSKIP0:
            phase0()
        if MAXPH >= 1:
            phaseA()
        if MAXPH >= 2:
            phaseB()
        if MAXPH >= 3:
            phaseC()
        if MAXPH >= 4:
            ffn_phase(0, [(0, [0, 1, 2, 3]), (1, list(range(4, 12))), (1, list(range(12, 21)))],
                      [dense_expert(0), dense_expert(1)], False, MT=9)
        if MAXPH >= 5:
            phaseE1()
        if MAXPH >= 6:
            phaseE2()
        if MAXPH >= 7:
            ffn_phase(1, [(0, [0, 1, 2, 3]), (1, list(range(4, 12))), (1, list(range(12, 20)))],
                      [moe_expert(e) for e in range(NE)], True)
        S.emit()
    return nc


def _bf16_round(a):
    return a


_PROG = {}


def kernel(x_prompt, x_sample, c, cache_k, cache_v, c_ctx, ada_w, ada_b, norm1_g, norm2_g,
           attn_w_qkv, attn_w_o, attn_q_norm, attn_k_norm, attn_lambda, attn_subln_g,
           pool_w, pool_scale, ffn_w_gu, ffn_w_down,
           moe_w_router, moe_b_router, moe_w_gu, moe_w_down):
    f = lambda a: np.ascontiguousarray(np.asarray(a, dtype=np.float32))
    x_prompt, x_sample, c, cache_k, cache_v, c_ctx = map(f, (x_prompt, x_sample, c, cache_k, cache_v, c_ctx))
    if "nc" not in _PROG:
        _PROG["nc"] = build_program()
    nc = _PROG["nc"]

    shared = dict(
        ada_w=f(ada_w), ada_b=f(ada_b), n1g=f(norm1_g), n2g=f(norm2_g),
        wqkv=f(attn_w_qkv).reshape(D, 3 * D), wo=f(attn_w_o).reshape(D, D),
        qng=f(attn_q_norm).reshape(1, HD), kng=f(attn_k_norm).reshape(1, HD),
        lamp=f(attn_lambda).reshape(1, 4 * HD), subg=f(attn_subln_g).reshape(1, 256),
        poolw=f(pool_w).reshape(4, 512, 512), pools=f(pool_scale).reshape(1, D),
        wgu=f(ffn_w_gu).reshape(D, 2 * DFF), wdn=f(ffn_w_down).reshape(DFF, D),
        wrT=np.ascontiguousarray(f(moe_w_router).reshape(D, NE).T), br=f(moe_b_router).reshape(1, NE),
        mgu=f(moe_w_gu).reshape(NE, D, 2 * DFE), mdn=f(moe_w_down).reshape(NE, DFE, D),
        ident=np.eye(128, dtype=np.float32),
    )
    GRID_W = 64
    nfreq = HD // 4
    freqs = (np.float32(10000.0) ** (-np.arange(nfreq, dtype=np.float32) / np.float32(nfreq))).astype(np.float32)
    windows = (2, 4, 8, 16)

    def pool_blocks(t0, L):
        out = np.zeros((128, 3, 4, 128), np.float32)
        t = np.arange(t0, t0 + 128)
        for g, w in enumerate(windows):
            lo = np.clip(t - w // 2, 0, L - 1)
            hi = np.clip(t + w // 2 - 1, 0, L - 1)
            cnt = (hi - lo + 1).astype(np.float32)
            for blk in range(3):
                s = np.arange(t0 + (blk - 1) * 128, t0 + blk * 128)
                m = ((s[:, None] >= lo[None, :]) & (s[:, None] <= hi[None, :])).astype(np.float32) / cnt[None, :]
                m = m - (s[:, None] == t[None, :]).astype(np.float32)
                out[:, blk, g, :] = m
        return out

    in_maps = []
    for core in range(8):
        b = core // 2
        s = core % 2
        own = np.arange(s * 2048, (s + 1) * 2048)
        if s == 0:
            halo = np.arange(2048, 2176)
            others = np.arange(2176, 4096)
        else:
            halo = np.arange(1920, 2048)
            others = np.arange(0, 1920)
        order = np.concatenate([own, halo, others])
        r = (order // GRID_W).astype(np.float32)
        col = (order % GRID_W).astype(np.float32)
        ang = np.stack([r[:, None] * freqs[None, :], col[:, None] * freqs[None, :]], axis=1).astype(np.float32)
        cs = np.cos(ang).astype(np.float32)
        sn = np.sin(ang).astype(np.float32)
        rc = np.stack([cs, cs], axis=2).reshape(4096, 128)
        rsn = np.stack([-sn, sn], axis=2).reshape(4096, 128)
        cond = np.stack([c_ctx, c[b]], axis=0)
        condT = np.ascontiguousarray(cond.reshape(2, 16, 128).transpose(2, 0, 1).reshape(128, 32))
        ap = np.zeros((20, 128, 3, 4, 128), np.float32)
        for i in range(4):
            ap[i] = pool_blocks((i % 2) * 128, 256)
        for i in range(16):
            ap[4 + i] = pool_blocks(s * 2048 + i * 128, 4096)
        m = dict(shared)
        m.update(
            xp=np.ascontiguousarray(x_prompt[2 * core:2 * core + 2].reshape(512, D)),
            xs=np.ascontiguousarray(x_sample[b][order]),
            condT=condT,
            ck=np.ascontiguousarray(cache_k[b, 0].reshape(PAST, D)),
            cv=np.ascontiguousarray(cache_v[b, 0].reshape(PAST, D)),
            ropeC=np.ascontiguousarray(rc), ropeS=np.ascontiguousarray(rsn),
            apool=ap.reshape(20, 128, 3 * 4 * 128),
        )
        in_maps.append({k: v for k, v in m.items() if k in _DECL})

    res = run_bass_kernel_spmd(nc, in_maps, core_ids=list(range(8)))
    _PROG['last'] = res
    y_prompt = np.zeros((16, 256, D), np.float32)
    y_sample = np.zeros((4, 4096, D), np.float32)
    state_k = np.zeros((16, 1, 256, NH, 256), np.float32)
    state_v = np.zeros((16, 1, 256, NH, 256), np.float32)
    for core in range(8):
        r = res.results[core]
        b = core // 2
        s = core % 2
        y_prompt[2 * core:2 * core + 2] = r["yp"].reshape(2, 256, D)
        y_sample[b, s * 2048:(s + 1) * 2048] = r["ys"]
        state_k[2 * core:2 * core + 2, 0] = r["sk"].reshape(2, 256, NH, 256)
        state_v[2 * core:2 * core + 2, 0] = r["sv"].reshape(2, 256, NH, 256)
    return (y_prompt, y_sample, state_k, state_v)
```
